# Optimizing a Trainium2 kernel written in Bass

```python
import jax, jax.numpy as jnp
from jax import lax
import numpy as np

D_MODEL = 1024
BATCH = 8
SEQ = 2048
DEPTH = 1
DEC_BATCH = 128
DEC_SEQ = 4
PAST_LEN = 16384
PAGE_SIZE = 128

MIX_WIDTH = D_MODEL
SSM_WIDTH = MIX_WIDTH // 2
CONV_CH = MIX_WIDTH - SSM_WIDTH
SSM_GROUP_CH = 16
SSM_GROUPS = SSM_WIDTH // SSM_GROUP_CH
SSM_STATE = 64
CONV_WIDTH = 31
CONV_BUF = CONV_WIDTH - 1
IN_PROJ = SSM_WIDTH + 2 * CONV_CH
N_EXPERT_GROUPS = 4
EXPERTS_PER_GROUP = 8
N_EXPERTS = N_EXPERT_GROUPS * EXPERTS_PER_GROUP
TOP_K = 2
D_EXPERT = D_MODEL // 2
EPS = 1e-6
DT_MIN = 0.001
DT_MAX = 0.1

kernel_name = "hymba_s5_conformer_hiermoe_decode_step"


def rms_norm(x, g):
    xf = x.astype(jnp.float32)
    y = xf * lax.rsqrt(jnp.mean(xf * xf, axis=-1, keepdims=True) + EPS)
    return y * g.astype(jnp.float32)


def layer_norm(x, g, b):
    xf = x.astype(jnp.float32)
    mu = jnp.mean(xf, axis=-1, keepdims=True)
    var = jnp.mean(jnp.square(xf - mu), axis=-1, keepdims=True)
    return (xf - mu) * lax.rsqrt(var + EPS) * g.astype(jnp.float32) + b.astype(jnp.float32)


def _complex_affine_combine(e1, e2):
    a1r, a1i, b1r, b1i = e1
    a2r, a2i, b2r, b2i = e2
    ar = a2r * a1r - a2i * a1i
    ai = a2r * a1i + a2i * a1r
    br = a2r * b1r - a2i * b1i + b2r
    bi = a2r * b1i + a2i * b1r + b2i
    return (ar, ai, br, bi)


def ssm_mixer(u, h0_re, h0_im, a_re, a_im, log_dt, b_re, b_im, c_re, c_im, d_skip, w_glu, b_glu):
    f32 = jnp.float32
    bsz, t, _ = u.shape
    uf = u.astype(f32).reshape(bsz, t, SSM_GROUPS, SSM_GROUP_CH)
    lam_re = jnp.minimum(a_re.astype(f32), -1e-4)
    lam_im = a_im.astype(f32)
    dt = jnp.exp(log_dt.astype(f32))[:, None]
    mag = jnp.exp(lam_re * dt)
    ab_re = mag * jnp.cos(lam_im * dt)
    ab_im = mag * jnp.sin(lam_im * dt)
    den = lam_re * lam_re + lam_im * lam_im
    num_re = ab_re - 1.0
    coef_re = (num_re * lam_re + ab_im * lam_im) / den
    coef_im = (ab_im * lam_re - num_re * lam_im) / den
    b_re = b_re.astype(f32)
    b_im = b_im.astype(f32)
    bb_re = coef_re[..., None] * b_re - coef_im[..., None] * b_im
    bb_im = coef_re[..., None] * b_im + coef_im[..., None] * b_re
    bu_re = jnp.einsum('btgh,gph->btgp', uf, bb_re)
    bu_im = jnp.einsum('btgh,gph->btgp', uf, bb_im)
    h0_re = h0_re.astype(f32)
    h0_im = h0_im.astype(f32)
    bu_re = bu_re.at[:, 0].add(ab_re * h0_re - ab_im * h0_im)
    bu_im = bu_im.at[:, 0].add(ab_re * h0_im + ab_im * h0_re)
    a_seq_re = jnp.broadcast_to(ab_re, (1, t) + ab_re.shape)
    a_seq_im = jnp.broadcast_to(ab_im, (1, t) + ab_im.shape)
    _, _, hr, hi = lax.associative_scan(_complex_affine_combine,
                                        (a_seq_re, a_seq_im, bu_re, bu_im), axis=1)
    y = (jnp.einsum('ghp,btgp->btgh', c_re.astype(f32), hr)
         - jnp.einsum('ghp,btgp->btgh', c_im.astype(f32), hi)
         + d_skip.astype(f32) * uf)
    y = jax.nn.gelu(y.reshape(bsz, t, SSM_WIDTH))
    y = y * jax.nn.sigmoid(y @ w_glu + b_glu)
    return y, hr[:, -1], hi[:, -1]


def conv_mixer(v, buf, w_dw, b_dw, ln_g, ln_b):
    f32 = jnp.float32
    a, g = jnp.split(v.astype(f32), 2, axis=-1)
    glu = a * jax.nn.sigmoid(g)
    ext = jnp.concatenate([buf.astype(f32), glu], axis=1)
    out = lax.conv_general_dilated(ext, w_dw.astype(f32)[:, None, :], window_strides=(1,),
                                   padding='VALID', dimension_numbers=('NWC', 'WIO', 'NWC'),
                                   feature_group_count=CONV_CH) + b_dw
    out = jax.nn.silu(layer_norm(out, ln_g, ln_b))
    return out, ext[:, -CONV_BUF:]


def hier_moe(h, w_rg, b_rg, w_re, b_re, w1, w3, w2):
    f32 = jnp.float32
    bsz, t, d = h.shape
    hf = h.reshape(bsz * t, d)
    p_grp = jax.nn.softmax((hf @ w_rg + b_rg).astype(f32), axis=-1)
    p_top, g_idx = lax.top_k(p_grp, 1)
    le = (jnp.einsum('nd,dge->nge', hf, w_re) + b_re).astype(f32)
    le_sel = jnp.take_along_axis(le, g_idx[:, :, None], axis=1)[:, 0]
    ev, ei = lax.top_k(le_sel, TOP_K)
    ew = jax.nn.softmax(ev, axis=-1) * p_top
    eidx = g_idx * EXPERTS_PER_GROUP + ei
    combine = jnp.sum(jax.nn.one_hot(eidx, N_EXPERTS, dtype=f32) * ew[..., None], axis=1)
    out = jnp.zeros((bsz * t, d), f32)
    for e in range(N_EXPERTS):
        hid = jax.nn.silu(hf @ w1[e]) * (hf @ w3[e])
        out = out + combine[:, e:e + 1] * (hid @ w2[e])
    return out.reshape(bsz, t, d)


def layer(x, c, h0_re, h0_im, conv_buf,
          w_ada, b_ada, g_norm_mix, w_in,
          ssm_a_re, ssm_a_im, ssm_log_dt, ssm_b_re, ssm_b_im, ssm_c_re, ssm_c_im, ssm_d,
          w_ssm_glu, b_ssm_glu, w_dw, b_dw, ln_conv_g, ln_conv_b,
          g_out_ssm, g_out_conv, w_out, g_norm_ffn,
          w_router_grp, b_router_grp, w_router_exp, b_router_exp,
          w_exp_gate, w_exp_up, w_exp_down):
    mod = jax.nn.silu(c.astype(jnp.float32)) @ w_ada + b_ada
    sh1, sc1, gt1, sh2, sc2, gt2 = jnp.split(mod[:, None, :], 6, axis=-1)
    n = rms_norm(x, g_norm_mix) * (1.0 + sc1) + sh1
    proj = n @ w_in
    ys, hr, hi = ssm_mixer(proj[..., :SSM_WIDTH], h0_re, h0_im, ssm_a_re, ssm_a_im, ssm_log_dt,
                           ssm_b_re, ssm_b_im, ssm_c_re, ssm_c_im, ssm_d, w_ssm_glu, b_ssm_glu)
    yc, new_buf = conv_mixer(proj[..., SSM_WIDTH:], conv_buf, w_dw, b_dw, ln_conv_g, ln_conv_b)
    merged = jnp.concatenate([rms_norm(ys, g_out_ssm), rms_norm(yc, g_out_conv)], axis=-1) @ w_out
    x = x + gt1 * merged
    n2 = rms_norm(x, g_norm_ffn) * (1.0 + sc2) + sh2
    x = x + gt2 * hier_moe(n2, w_router_grp, b_router_grp, w_router_exp, b_router_exp,
                           w_exp_gate, w_exp_up, w_exp_down)
    return x, hr, hi, new_buf


def setup_inputs(seed: int = 0) -> dict:
    key = jax.random.key(seed)
    ks = jax.random.split(key, 40)
    f32 = jnp.float32
    nrm = lambda k, shape, s: jax.random.normal(k, shape, f32) * s
    L, D, G, P, H = DEPTH, D_MODEL, SSM_GROUPS, SSM_STATE, SSM_GROUP_CH
    n_idx = jnp.arange(P, dtype=f32)
    inp = {}
    inp['x_prompt'] = nrm(ks[0], (BATCH, SEQ, D), 1.0)
    inp['x_sample'] = nrm(ks[1], (DEC_BATCH, DEC_SEQ, D), 1.0)
    inp['c_prompt'] = nrm(ks[2], (BATCH, D), 1.0)
    inp['c_sample'] = nrm(ks[3], (DEC_BATCH, D), 1.0)
    inp['state_ssm_re'] = nrm(ks[4], (L, DEC_BATCH, G, P), 0.5)
    inp['state_ssm_im'] = nrm(ks[5], (L, DEC_BATCH, G, P), 0.5)
    inp['cache_conv'] = nrm(ks[6], (L, DEC_BATCH, CONV_BUF, CONV_CH), 0.5)
    inp['w_ada'] = nrm(ks[7], (L, D, 6 * D), 0.3 * D ** -0.5)
    inp['b_ada'] = nrm(ks[8], (L, 6 * D), 0.02)
    inp['g_norm_mix'] = 1.0 + nrm(ks[9], (L, D), 0.02)
    inp['w_in'] = nrm(ks[10], (L, D, IN_PROJ), D ** -0.5)
    inp['ssm_a_re'] = -0.5 + nrm(ks[11], (L, G, P), 0.01)
    inp['ssm_a_im'] = jnp.pi * n_idx + nrm(ks[12], (L, G, P), 0.01)
    inp['ssm_log_dt'] = jax.random.uniform(ks[13], (L, G), f32, float(np.log(DT_MIN)), float(np.log(DT_MAX)))
    inp['ssm_b_re'] = nrm(ks[14], (L, G, P, H), (2.0 * H) ** -0.5)
    inp['ssm_b_im'] = nrm(ks[15], (L, G, P, H), (2.0 * H) ** -0.5)
    inp['ssm_c_re'] = nrm(ks[16], (L, G, H, P), (2.0 * P) ** -0.5)
    inp['ssm_c_im'] = nrm(ks[17], (L, G, H, P), (2.0 * P) ** -0.5)
    inp['ssm_d'] = nrm(ks[18], (L, G, H), 1.0)
    inp['w_ssm_glu'] = nrm(ks[19], (L, SSM_WIDTH, SSM_WIDTH), SSM_WIDTH ** -0.5)
    inp['b_ssm_glu'] = nrm(ks[20], (L, SSM_WIDTH), 0.02)
    inp['w_dw'] = nrm(ks[21], (L, CONV_WIDTH, CONV_CH), CONV_WIDTH ** -0.5)
    inp['b_dw'] = nrm(ks[22], (L, CONV_CH), 0.02)
    inp['ln_conv_g'] = 1.0 + nrm(ks[23], (L, CONV_CH), 0.02)
    inp['ln_conv_b'] = nrm(ks[24], (L, CONV_CH), 0.02)
    inp['g_out_ssm'] = 1.0 + nrm(ks[25], (L, SSM_WIDTH), 0.02)
    inp['g_out_conv'] = 1.0 + nrm(ks[26], (L, CONV_CH), 0.02)
    inp['w_out'] = nrm(ks[27], (L, MIX_WIDTH, D), MIX_WIDTH ** -0.5)
    inp['g_norm_ffn'] = 1.0 + nrm(ks[28], (L, D), 0.02)
    inp['w_router_grp'] = nrm(ks[29], (L, D, N_EXPERT_GROUPS), D ** -0.5)
    inp['b_router_grp'] = nrm(ks[30], (L, N_EXPERT_GROUPS), 0.01)
    inp['w_router_exp'] = nrm(ks[31], (L, D, N_EXPERT_GROUPS, EXPERTS_PER_GROUP), D ** -0.5)
    inp['b_router_exp'] = nrm(ks[32], (L, N_EXPERT_GROUPS, EXPERTS_PER_GROUP), 0.01)
    inp['w_exp_gate'] = nrm(ks[33], (L, N_EXPERTS, D, D_EXPERT), D ** -0.5)
    inp['w_exp_up'] = nrm(ks[34], (L, N_EXPERTS, D, D_EXPERT), D ** -0.5)
    inp['w_exp_down'] = nrm(ks[35], (L, N_EXPERTS, D_EXPERT, D), D_EXPERT ** -0.5)
    inp['g_final'] = 1.0 + nrm(ks[36], (D,), 0.02)
    return inp


def reference(x_prompt, x_sample, c_prompt, c_sample, state_ssm_re, state_ssm_im, cache_conv,
              w_ada, b_ada, g_norm_mix, w_in,
              ssm_a_re, ssm_a_im, ssm_log_dt, ssm_b_re, ssm_b_im, ssm_c_re, ssm_c_im, ssm_d,
              w_ssm_glu, b_ssm_glu, w_dw, b_dw, ln_conv_g, ln_conv_b,
              g_out_ssm, g_out_conv, w_out, g_norm_ffn,
              w_router_grp, b_router_grp, w_router_exp, b_router_exp,
              w_exp_gate, w_exp_up, w_exp_down, g_final):
    stacked = (w_ada, b_ada, g_norm_mix, w_in,
               ssm_a_re, ssm_a_im, ssm_log_dt, ssm_b_re, ssm_b_im, ssm_c_re, ssm_c_im, ssm_d,
               w_ssm_glu, b_ssm_glu, w_dw, b_dw, ln_conv_g, ln_conv_b,
               g_out_ssm, g_out_conv, w_out, g_norm_ffn,
               w_router_grp, b_router_grp, w_router_exp, b_router_exp,
               w_exp_gate, w_exp_up, w_exp_down)
    xp, xs = x_prompt, x_sample
    zero_h = jnp.zeros((BATCH, SSM_GROUPS, SSM_STATE), jnp.float32)
    zero_buf = jnp.zeros((BATCH, CONV_BUF, CONV_CH), jnp.float32)
    p_re, p_im, p_buf, s_re, s_im, s_buf = [], [], [], [], [], []
    for l in range(DEPTH):
        lp = [w[l] for w in stacked]
        xp, hr, hi, nb = layer(xp, c_prompt, zero_h, zero_h, zero_buf, *lp)
        p_re.append(hr); p_im.append(hi); p_buf.append(nb)
        xs, hr, hi, nb = layer(xs, c_sample, state_ssm_re[l], state_ssm_im[l], cache_conv[l], *lp)
        s_re.append(hr); s_im.append(hi); s_buf.append(nb)
    y_prompt = rms_norm(xp, g_final)
    y_sample = rms_norm(xs, g_final)
    return (y_prompt, y_sample,
            jnp.stack(p_re), jnp.stack(p_im), jnp.stack(p_buf),
            jnp.stack(s_re), jnp.stack(s_im), jnp.stack(s_buf))
```

```python
import math
from contextlib import ExitStack

import numpy as np
import concourse.bass as bass
import concourse.mybir as mybir
from concourse.bass_utils import run_bass_kernel_spmd

F32 = mybir.dt.float32
BF16 = mybir.dt.bfloat16
I32 = mybir.dt.int32
AF = mybir.ActivationFunctionType
ALU = mybir.AluOpType
AX = mybir.AxisListType

NCORES = 8
D = 1024
SEQ = 2048
NS = 16
TS = 4
NTOK = SEQ + NS * TS
NT = 17
G = 32
P = 64
H = 16
CW = 31
CB = 30
NE = 32
DE = 512
EPS = 1e-6
TWO_PI = 2.0 * math.pi
CW1 = 6.28125
CW2 = TWO_PI - 6.28125
BLKS = [(0, 512), (512, 512), (1024, 512), (1536, 512), (2048, 64)]
CAP = 512
NJ = CAP // 128


def tile_rows(i):
    return 128 if i < 16 else 64


class Buf:
    __slots__ = ("name", "w", "r")

    def __init__(self, name):
        self.name = name
        self.w = None
        self.r = []


class S:
    def __init__(self, nc):
        self.nc = nc
        self.eng = {"pe": nc.tensor, "act": nc.scalar, "dve": nc.vector,
                    "pool": nc.gpsimd, "sp": nc.sync}
        self.sems = {}
        self.cnt = {}
        for k in self.eng:
            self.sems[k] = nc.alloc_semaphore("prog_" + k)
            self.cnt[k] = 0
        self.waited = {k: {} for k in self.eng}
        self.bufs = {}

    def _B(self, x):
        if isinstance(x, Buf):
            return x
        b = self.bufs.get(x)
        if b is None:
            b = Buf(x)
            self.bufs[x] = b
        return b

    def _sem(self, name):
        key = "dma_" + name
        if key not in self.sems:
            self.sems[key] = self.nc.alloc_semaphore(key)
            self.cnt[key] = 0
        return key

    def _wait(self, e, key, val):
        if val <= 0:
            return
        w = self.waited[e]
        if w.get(key, 0) >= val:
            return
        w[key] = val
        self.eng[e].wait_ge(self.sems[key], val)

    def _deps(self, e, reads, writes, own=None):
        need = {}

        def want(k, v):
            if need.get(k, 0) < v:
                need[k] = v

        for b in reads:
            b = self._B(b)
            if b.w is not None:
                want(b.w[0], b.w[1])
        same_ok = e in ("act", "dve", "pool")
        for b in writes:
            b = self._B(b)
            if b.w is not None and b.w[0] != own and (b.w[0] != e or same_ok):
                want(b.w[0], b.w[1])
            for (k, v) in b.r:
                if k != e or same_ok:
                    want(k, v)
        for k, v in need.items():
            self._wait(e, k, v)

    def _rec(self, key, tgt, reads, writes):
        for b in reads:
            self._B(b).r.append((key, tgt))
        for b in writes:
            b = self._B(b)
            b.w = (key, tgt)
            b.r = []

    def op(self, e, fn, reads=(), writes=(), signal=True):
        self._deps(e, reads, writes)
        ins = fn(self.eng[e])
        tgt = self.cnt[e] + 1
        if signal:
            ins.then_inc(self.sems[e], 1)
            self.cnt[e] = tgt
        self._rec(e, tgt, reads, writes)
        return ins

    def dma(self, q, out, in_, sem, reads=(), writes=(), **kw):
        key = self._sem(sem)
        self._deps(q, reads, writes, own=key)
        ins = self.eng[q].dma_start(out=out, in_=in_, **kw)
        ins.then_inc(self.sems[key], 16)
        self.cnt[key] += 16
        self._rec(key, self.cnt[key], reads, writes)
        return ins

    def dma_custom(self, q, fn, sem, reads=(), writes=()):
        key = self._sem(sem)
        self._deps(q, reads, writes, own=key)
        ins = fn(self.eng[q])
        ins.then_inc(self.sems[key], 16)
        self.cnt[key] += 16
        self._rec(key, self.cnt[key], reads, writes)
        return ins

    def commit_group(self, names, sem):
        key = "dma_" + sem
        for n in names:
            self._B(n).w = (key, self.cnt[key])

    def barrier(self):
        for e in self.eng:
            for k in self.sems:
                if k != e:
                    self._wait(e, k, self.cnt[k])

    def finish(self, e="sp"):
        for k in self.sems:
            if k != e:
                self._wait(e, k, self.cnt[k])


IN_SPECS = [
    ("x_all", [NTOK, D]), ("c_all", [17, D]),
    ("h0s", [128, G, NS]),
    ("cachef", [128, 4, NS, CB]),
    ("w_ada", [D, 6 * D]), ("b_ada", [1, 6 * D]),
    ("g_mix", [1, D]), ("g_ffn", [1, D]), ("g_fin", [1, D]),
    ("w_in", [D, 1536]), ("w_out", [D, D]), ("w_glu", [512, 512]),
    ("are2", [128, G]), ("aim2", [128, G]), ("ldt2", [128, G]),
    ("bx1", [128, G, H]), ("bx2", [128, G, H]),
    ("ct1", [128, G, H]), ("ct2", [128, G, H]),
    ("vec4", [128, 7, 4]),
    ("wdwT", [128, 4, CW]),
    ("w_rt", [D, 36]), ("b_rt", [1, 36]),
    ("w1", [NE, D, DE]), ("w3", [NE, D, DE]), ("w2", [NE, DE, D]),
    ("c_ident", [128, 128]), ("c_swp", [128, 128]), ("c_rowmask", [128, 8]),
    ("c_sgn", [128, 1]), ("c_jidx", [128, 576]), ("c_m01", [128, 64]),
    ("c_ltri", [128, 128]), ("c_eoff", [128, 32]),
]
OUT_SPECS = [
    ("y_all", [NTOK, D]),
    ("st_p", [128, G]), ("st_s", [128, G, NS]),
    ("cc_p", [128, 4, CB]), ("cc_s", [128, 4, NS, CB]),
]


def build(dbg=False, n_exp=NE):
    nc = bass.Bass("TRN2", target_bir_lowering=False)
    I = {}
    for name, shp in IN_SPECS:
        I[name] = nc.dram_tensor(name, shp, F32, kind="ExternalInput").ap()
    O = {}
    for name, shp in OUT_SPECS:
        O[name] = nc.dram_tensor(name, shp, F32, kind="ExternalOutput").ap()
    mod_scr = nc.dram_tensor("mod_scr", [17, 6 * D], F32, kind="Internal").ap()
    x1_scr = nc.dram_tensor("x1_scr", [NTOK, D], F32, kind="Internal").ap()
    n2_scr = nc.dram_tensor("n2_scr", [NTOK, D], BF16, kind="Internal").ap()
    ys_f_scr = nc.dram_tensor("ys_f_scr", [128, 4, NTOK], F32, kind="Internal").ap()
    xs_scr = nc.dram_tensor("xs_scr", [NE * CAP, D], BF16, kind="Internal").ap()
    ys_scr = nc.dram_tensor("ys_scr", [NE * CAP, D], F32, kind="Internal").ap()
    DBG = {}

    def dbg_out(name, shape):
        if not dbg:
            return None
        DBG[name] = nc.dram_tensor("dbg_" + name, shape, F32, kind="ExternalOutput").ap()
        return DBG[name]

    s = S(nc)
    glob = ExitStack()

    import contextlib

    @contextlib.contextmanager
    def phase():
        with ExitStack() as es_:
            yield es_
            s.barrier()

    def sbt(es, name, shape, dt=F32):
        return es.enter_context(nc.sbuf_tensor("s_" + name, shape, dt))

    def pst(es, name, shape, dt=F32):
        return es.enter_context(nc.psum_tensor("p_" + name, shape, dt))

    with glob:
        ident = sbt(glob, "ident", [128, 128])
        identb = sbt(glob, "identb", [128, 128], BF16)
        sgn = sbt(glob, "sgn", [128, 1])
        epsc = sbt(glob, "epsc", [128, 1])
        halfpi = sbt(glob, "halfpi", [128, 1])
        ones32 = sbt(glob, "ones32", [128, 128])
        vec4 = sbt(glob, "vec4", [128, 7, 4])
        logit = sbt(glob, "logit", [128, NT, 36])
        mix_es = ExitStack()
        glob.enter_context(mix_es)
        mergedT = sbt(mix_es, "mergedT", [128, 8, NTOK], BF16)
        uT = sbt(mix_es, "uT", [128, 4, NTOK], BF16)
        s.dma("sp", ident[:], I["c_ident"], "c0", writes=["ident"])
        s.dma("sp", sgn[:], I["c_sgn"], "c1", writes=["sgn"])
        s.dma("sp", vec4[:], I["vec4"], "c2", writes=["vec4"])
        s.op("dve", lambda e: e.tensor_copy(identb[:], ident[:]), reads=["ident"], writes=["identb"])
        s.op("dve", lambda e: e.memset(epsc[:], EPS), writes=["epsc"])
        s.op("dve", lambda e: e.memset(halfpi[:], math.pi / 2), writes=["halfpi"])
        s.op("dve", lambda e: e.memset(ones32[:], 1.0), writes=["ones32"])
        V_D, V_BGLU, V_BDW, V_LNG, V_LNB, V_GOS, V_GOC = range(7)


        with phase() as es:
            cT = sbt(es, "cT", [128, 8, 17])
            crow = sbt(es, "crow", [17, D])
            bada = sbt(es, "bada", [17, 6 * D])
            modsb = sbt(es, "modsb", [17, 6 * D])
            wa = [sbt(es, "wa%d" % i, [128, 8, 512], BF16) for i in range(3)]
            cTb = sbt(es, "cTb", [128, 8, 17], BF16)
            mp = [pst(es, "mp%d" % i, [17, 512]) for i in range(2)]
            tp = pst(es, "tp0", [128, 8, 17])
            s.dma("sp", crow[:], I["c_all"], "p0a", writes=["crow"])
            s.dma("sp", bada[:], I["b_ada"].to_broadcast([17, 6 * D]), "p0b", writes=["bada"])
            s.op("act", lambda e: e.activation(crow[:], crow[:], AF.Silu), reads=["crow"], writes=["crow"])
            for k in range(8):
                s.op("pe", lambda e: e.transpose(tp[:, k, :], crow[:, k * 128:(k + 1) * 128], ident[0:17, 0:17]),
                     reads=["crow", "ident"], writes=["tp0"])
            s.op("dve", lambda e: e.tensor_copy(cTb[:], tp[:]), reads=["tp0"], writes=["cT"])
            for j in range(12):
                wb_ = wa[j % 3]
                wn = "wa%d" % (j % 3)
                s.dma("pool", wb_[:], I["w_ada"][:, j * 512:(j + 1) * 512].rearrange("(k p) n -> p k n", p=128),
                      wn, writes=[wn])
                mpp = mp[j % 2]
                mn = "mp%d" % (j % 2)
                for k in range(8):
                    s.op("pe", lambda e: e.matmul(mpp[:], cTb[:, k, :], wb_[:, k, :], start=(k == 0), stop=(k == 7)),
                         reads=["cT", wn], writes=[mn], signal=(k == 7))
                s.op("dve", lambda e: e.tensor_tensor(modsb[:, j * 512:(j + 1) * 512], mpp[:], bada[:, j * 512:(j + 1) * 512], ALU.add),
                     reads=[mn, "bada"], writes=["modsb"])
            s.dma("sp", mod_scr, modsb[:], "p0c", reads=["modsb"], writes=["mod_scr"])

        def load_mod(es, which, names, gname=None):
            res = {}
            for idx, nm in zip(which, names):
                tp_ = sbt(es, "modP_" + nm, [128, D])
                ts_ = sbt(es, "modS_" + nm, [64, D])
                s.dma("sp", tp_[:], mod_scr[0:1, idx * D:(idx + 1) * D].to_broadcast([128, D]), "mdp" + nm,
                      reads=["mod_scr"], writes=["modP_" + nm])
                for b in range(NS):
                    s.dma("sp", ts_[b * TS:(b + 1) * TS, :], mod_scr[1 + b:2 + b, idx * D:(idx + 1) * D].to_broadcast([TS, D]), "mds" + nm,
                          reads=["mod_scr"], writes=["modS_" + nm])
                res[nm] = (tp_, ts_)
            return res

        gluB_es = ExitStack()
        glob.enter_context(gluB_es)
        gluB = sbt(gluB_es, "gluB", [128, 4, CB + SEQ], BF16)
        gluS = sbt(gluB_es, "gluS", [128, 4, NS, CB + TS], BF16)
        gluT = sbt(gluB_es, "gluT", [128, 4, CB], F32)
        gluST = sbt(gluB_es, "gluST", [128, 4, NS, CB], F32)
        with phase() as es:
            md = load_mod(es, [0, 1], ["sh1", "sc1"])
            gmix = sbt(es, "gmix", [128, D])
            s.dma("sp", gmix[:], I["g_mix"].to_broadcast([128, D]), "gm", writes=["gmix"])
            for (tl, nm, R) in ((md["sc1"][0], "modP_sc1", 128), (md["sc1"][1], "modS_sc1", 64)):
                s.op("dve", lambda e: e.scalar_tensor_tensor(tl[0:R, :], tl[0:R, :], 1.0, gmix[0:R, :], ALU.add, ALU.mult),
                     reads=[nm, "gmix"], writes=[nm])
            w_in_b = sbt(es, "w_in_b", [128, 8, 1536], BF16)
            s.dma("pool", w_in_b[:], I["w_in"].rearrange("(k p) n -> p k n", p=128), "win", writes=["w_in_b"])
            nTs = [sbt(es, "nT%d" % i, [128, 8, 512], BF16) for i in range(2)]
            xt = [sbt(es, "xt%d" % i, [128, D]) for i in range(3)]
            nt_ = [sbt(es, "nt%d" % i, [128, D]) for i in range(3)]
            ssq = sbt(es, "ssq", [128, NT])
            rstd = sbt(es, "rstd", [128, NT])
            cst = sbt(es, "cst", [128, 4, NS, CB])
            sig = [sbt(es, "sigA%d" % i, [128, 512]) for i in range(2)]
            gl = [sbt(es, "glA%d" % i, [128, 512]) for i in range(2)]
            tpp = [pst(es, "tpA%d" % i, [128, 8, 128]) for i in range(2)]
            pp = [pst(es, "ppA%d" % i, [128, 512]) for i in range(4)]
            s.op("pool", lambda e: e.memset(gluB[:, :, 0:CB], 0.0), writes=["gluB"])
            s.dma("sp", cst[:], I["cachef"], "cst", writes=["cst"])
            s.op("pool", lambda e: e.tensor_copy(gluS[:, :, :, 0:CB], cst[:]), reads=["cst"], writes=["gluS"])
            s.op("pool", lambda e: e.tensor_copy(gluST[:, :, :, 0:CB - TS], cst[:, :, :, TS:CB]), reads=["cst"], writes=["gluST"])
            cnt = 0
            for bi, (b0, bw) in enumerate(BLKS):
                nT = nTs[bi % 2]
                nTn = "nT%d" % (bi % 2)
                tiles = range(bi * 4, bi * 4 + 4) if bi < 4 else [16]
                for i in tiles:
                    R = tile_rows(i)
                    c0 = i * 128
                    l0 = c0 - b0
                    xb, xn = xt[i % 3], "xt%d" % (i % 3)
                    nb, nn = nt_[i % 3], "nt%d" % (i % 3)
                    G1 = md["sc1"][0 if i < 16 else 1]
                    S1 = md["sh1"][0 if i < 16 else 1]
                    g1n = "modP_sc1" if i < 16 else "modS_sc1"
                    s1n = "modP_sh1" if i < 16 else "modS_sh1"
                    s.dma("sp", xb[0:R, :], I["x_all"][c0:c0 + R, :], xn, writes=[xn])
                    s.op("act", lambda e: e.activation(nb[0:R, :], xb[0:R, :], AF.Square, accum_out=ssq[0:R, i:i + 1]),
                         reads=[xn], writes=[nn, "ssq"])
                    s.op("act", lambda e: e.activation(rstd[0:R, i:i + 1], ssq[0:R, i:i + 1], AF.Sqrt, scale=1.0 / D, bias=epsc[0:R, :]),
                         reads=["ssq", "epsc"], writes=["rstd"])
                    s.op("dve", lambda e: e.reciprocal(rstd[0:R, i:i + 1], rstd[0:R, i:i + 1]), reads=["rstd"], writes=["rstd"])
                    s.op("dve", lambda e: e.scalar_tensor_tensor(nb[0:R, :], xb[0:R, :], rstd[0:R, i:i + 1], G1[0:R, :], ALU.mult, ALU.mult),
                         reads=[xn, "rstd", g1n], writes=[nn])
                    s.op("dve", lambda e: e.tensor_tensor(nb[0:R, :], nb[0:R, :], S1[0:R, :], ALU.add), reads=[nn, s1n], writes=[nn])
                    tpb, tpn = tpp[i % 2], "tpA%d" % (i % 2)
                    for k in range(8):
                        s.op("pe", lambda e: e.transpose(tpb[:, k, 0:R], nb[0:R, k * 128:(k + 1) * 128], ident[0:R, 0:R]),
                             reads=[nn, "ident"], writes=[tpn], signal=(k == 7))
                    s.op("act", lambda e: e.copy(nT[:, :, l0:l0 + R], tpb[:, :, 0:R]), reads=[tpn], writes=[nTn])
                for m in range(4):
                    pb, pn = pp[cnt % 4], "ppA%d" % (cnt % 4)
                    cnt += 1
                    for k in range(8):
                        s.op("pe", lambda e: e.matmul(pb[:, 0:bw], w_in_b[:, k, m * 128:(m + 1) * 128], nT[:, k, 0:bw],
                                                      start=(k == 0), stop=(k == 7)),
                             reads=["w_in_b", nTn], writes=[pn], signal=(k == 7))
                    s.op("act", lambda e: e.copy(uT[:, m, b0:b0 + bw], pb[:, 0:bw]), reads=[pn], writes=["uT"])
                for m in range(4):
                    pa, pan = pp[cnt % 4], "ppA%d" % (cnt % 4)
                    cnt += 1
                    pg, pgn = pp[cnt % 4], "ppA%d" % (cnt % 4)
                    cnt += 1
                    for k in range(8):
                        s.op("pe", lambda e: e.matmul(pa[:, 0:bw], w_in_b[:, k, 512 + m * 128:512 + (m + 1) * 128], nT[:, k, 0:bw],
                                                      start=(k == 0), stop=(k == 7)),
                             reads=["w_in_b", nTn], writes=[pan], signal=(k == 7))
                    for k in range(8):
                        s.op("pe", lambda e: e.matmul(pg[:, 0:bw], w_in_b[:, k, 1024 + m * 128:1024 + (m + 1) * 128], nT[:, k, 0:bw],
                                                      start=(k == 0), stop=(k == 7)),
                             reads=["w_in_b", nTn], writes=[pgn], signal=(k == 7))
                    sg, sgn_ = sig[m % 2], "sigA%d" % (m % 2)
                    gg, ggn = gl[m % 2], "glA%d" % (m % 2)
                    s.op("act", lambda e: e.activation(sg[:, 0:bw], pg[:, 0:bw], AF.Sigmoid), reads=[pgn], writes=[sgn_])
                    s.op("dve", lambda e: e.tensor_tensor(gg[:, 0:bw], pa[:, 0:bw], sg[:, 0:bw], ALU.mult), reads=[pan, sgn_], writes=[ggn])
                    if b0 < SEQ:
                        s.op("act", lambda e: e.copy(gluB[:, m, CB + b0:CB + b0 + bw], gg[:, 0:bw]), reads=[ggn], writes=["gluB"])
                        if b0 + bw == SEQ:
                            s.op("pool", lambda e: e.tensor_copy(gluT[:, m, :], gg[:, bw - CB:bw]), reads=[ggn], writes=["gluT"])
                    else:
                        s.op("pool", lambda e: e.tensor_copy(gluS[:, m, :, CB:CB + TS], gg[:, 0:bw].rearrange("p (b t) -> p b t", t=TS)),
                             reads=[ggn], writes=["gluS"])
                        s.op("pool", lambda e: e.tensor_copy(gluST[:, m, :, CB - TS:CB], gg[:, 0:bw].rearrange("p (b t) -> p b t", t=TS)),
                             reads=[ggn], writes=["gluST"])
            s.dma("sp", O["cc_p"], gluT[:], "occp", reads=["gluT"], writes=["o_cc_p"])
            s.dma("sp", O["cc_s"], gluST[:], "occs", reads=["gluST"], writes=["o_cc_s"])
            if dbg:
                d_uT = dbg_out("uT", [128, 4, NTOK])
                dtmp = sbt(es, "dtmp", [128, 4, NTOK])
                s.op("dve", lambda e: e.tensor_copy(dtmp[:], uT[:]), reads=["uT"], writes=["dtmp"])
                s.dma("sp", d_uT, dtmp[:], "dbg0", reads=["dtmp"], writes=["d_uT"])

        with phase() as es:
            wdw = sbt(es, "wdw", [128, 4, CW])
            s.dma("sp", wdw[:], I["wdwT"], "wdw", writes=["wdw"])
            zt = sbt(es, "zt", [128, 2048], BF16)
            s.op("pool", lambda e: e.memset(zt[:], 0.0), writes=["zt"])
            xs_v = xs_scr.rearrange("(p q) d -> p (q d)", p=128)
            for q_ in range(NE * CAP // 128 // 2):
                s.dma("sp", xs_v[:, q_ * 2048:(q_ + 1) * 2048], zt[:], "zf", reads=["zt"], writes=["xs_zf"])

            diag = sbt(es, "diag", [128, 4, CW, 128], BF16)
            for m in range(4):
                for k in range(CW):
                    if (m * CW + k) % 2 == 0:
                        s.op("dve", lambda e: e.tensor_scalar(diag[:, m, k, :], ident[:], wdw[:, m, k:k + 1], None, ALU.mult),
                             reads=["ident", "wdw"], writes=["diag%d_%d" % (m, k)])
                    else:
                        s.op("act", lambda e: e.activation(diag[:, m, k, :], ident[:], AF.Copy, scale=wdw[:, m, k:k + 1]),
                             reads=["ident", "wdw"], writes=["diag%d_%d" % (m, k)])
            cps = [pst(es, "cps%d" % i, [128, 512]) for i in range(4)]
            stp = [pst(es, "stp%d" % i, [128, 512]) for i in range(3)]
            xc = [sbt(es, "xc%d" % i, [128, 512]) for i in range(4)]
            xq = [sbt(es, "xq%d" % i, [128, 512]) for i in range(4)]
            mu = sbt(es, "mu", [128, 512])
            var = sbt(es, "var", [128, 512])
            rs = sbt(es, "rsB", [128, 512])
            rs2 = sbt(es, "rs2B", [128, 512])
            for (b0, bw) in BLKS:
                for m in range(4):
                    for k in range(CW):
                        if b0 < SEQ:
                            rhs = gluB[:, m, b0 + k:b0 + k + bw]
                            rd = "gluB"
                        else:
                            rhs = gluS[:, m, :, k:k + TS]
                            rd = "gluS"
                        outp = cps[m][:, 0:bw] if b0 < SEQ else cps[m][:, 0:bw].rearrange("p (b t) -> p b t", t=TS)
                        s.op("pe", lambda e: e.matmul(outp, diag[:, m, k, :], rhs, start=(k == 0), stop=(k == CW - 1)),
                             reads=["diag%d_%d" % (m, k), rd], writes=["cps%d" % m], signal=(k == CW - 1))
                    s.op("act", lambda e: e.activation(xc[m][:, 0:bw], cps[m][:, 0:bw], AF.Identity, bias=vec4[:, V_BDW, m:m + 1]),
                         reads=["cps%d" % m, "vec4"], writes=["xc%d" % m])
                    s.op("pool", lambda e: e.tensor_tensor(xq[m][:, 0:bw], xc[m][:, 0:bw], xc[m][:, 0:bw], ALU.mult),
                         reads=["xc%d" % m], writes=["xq%d" % m])
                for m in range(4):
                    s.op("pe", lambda e: e.matmul(stp[0][:, 0:bw], ones32[:], xc[m][:, 0:bw], start=(m == 0), stop=(m == 3)),
                         reads=["ones32", "xc%d" % m], writes=["stp0"], signal=(m == 3))
                for m in range(4):
                    s.op("pe", lambda e: e.matmul(stp[1][:, 0:bw], ones32[:], xq[m][:, 0:bw], start=(m == 0), stop=(m == 3)),
                         reads=["ones32", "xq%d" % m], writes=["stp1"], signal=(m == 3))
                s.op("act", lambda e: e.mul(mu[:, 0:bw], stp[0][:, 0:bw], 1.0 / 512), reads=["stp0"], writes=["mu"])
                s.op("dve", lambda e: e.tensor_tensor(var[:, 0:bw], mu[:, 0:bw], mu[:, 0:bw], ALU.mult), reads=["mu"], writes=["var"])
                s.op("dve", lambda e: e.scalar_tensor_tensor(var[:, 0:bw], stp[1][:, 0:bw], 1.0 / 512, var[:, 0:bw], ALU.mult, ALU.subtract),
                     reads=["stp1", "var"], writes=["var"])
                s.op("act", lambda e: e.activation(rs[:, 0:bw], var[:, 0:bw], AF.Sqrt, bias=epsc[:, :]), reads=["var", "epsc"], writes=["rsB"])
                s.op("dve", lambda e: e.reciprocal(rs[:, 0:bw], rs[:, 0:bw]), reads=["rsB"], writes=["rsB"])
                for m in range(4):
                    s.op("dve", lambda e: e.tensor_tensor(xc[m][:, 0:bw], xc[m][:, 0:bw], mu[:, 0:bw], ALU.subtract),
                         reads=["xc%d" % m, "mu"], writes=["xc%d" % m])
                    s.op("dve", lambda e: e.tensor_tensor(xc[m][:, 0:bw], xc[m][:, 0:bw], rs[:, 0:bw], ALU.mult),
                         reads=["xc%d" % m, "rsB"], writes=["xc%d" % m])
                    s.op("act", lambda e: e.activation(xc[m][:, 0:bw], xc[m][:, 0:bw], AF.Silu, scale=vec4[:, V_LNG, m:m + 1], bias=vec4[:, V_LNB, m:m + 1]),
                         reads=["xc%d" % m, "vec4"], writes=["xc%d" % m])
                    s.op("pool", lambda e: e.tensor_tensor(xq[m][:, 0:bw], xc[m][:, 0:bw], xc[m][:, 0:bw], ALU.mult),
                         reads=["xc%d" % m], writes=["xq%d" % m])
                for m in range(4):
                    s.op("pe", lambda e: e.matmul(stp[2][:, 0:bw], ones32[:], xq[m][:, 0:bw], start=(m == 0), stop=(m == 3)),
                         reads=["ones32", "xq%d" % m], writes=["stp2"], signal=(m == 3))
                s.op("act", lambda e: e.activation(rs2[:, 0:bw], stp[2][:, 0:bw], AF.Sqrt, scale=1.0 / 512, bias=epsc[:, :]),
                     reads=["stp2", "epsc"], writes=["rs2B"])
                s.op("dve", lambda e: e.reciprocal(rs2[:, 0:bw], rs2[:, 0:bw]), reads=["rs2B"], writes=["rs2B"])
                for m in range(4):
                    s.op("dve", lambda e: e.scalar_tensor_tensor(mergedT[:, 4 + m, b0:b0 + bw], xc[m][:, 0:bw], vec4[:, V_GOC, m:m + 1], rs2[:, 0:bw], ALU.mult, ALU.mult),
                         reads=["xc%d" % m, "vec4", "rs2B"], writes=["mergedT"])
        s.barrier()
        gluB_es.close()

        with phase() as es:
            BBp = sbt(es, "BBp", [128, G, 128], BF16); BBq = sbt(es, "BBq", [128, G, 128], BF16)
            CAp = sbt(es, "CAp", [128, G, 128], BF16); CBp = sbt(es, "CBp", [128, G, 128], BF16)
            dgD = sbt(es, "dgD", [128, 4, 128], BF16)
            swp = sbt(es, "swp", [128, 128]); jidx = sbt(es, "jidx", [128, 576]); m01 = sbt(es, "m01", [128, 64])
            h0s = sbt(es, "h0s", [128, G, NS])
            mag = sbt(es, "mag", [128, G]); th = sbt(es, "th", [128, G])
            hcar = sbt(es, "hcar", [128, 5, G])
            stS = sbt(es, "stS", [128, G, NS])
            ysSt = [sbt(es, "ysSt%d" % i, [128, 512]) for i in range(2)]
            ysB = sbt(es, "ysB", [128, 4, NTOK], BF16)
            wgl = sbt(es, "wgl", [128, 4, 512], BF16)
            pre_es = ExitStack()
            are = sbt(pre_es, "are", [128, G]); aim = sbt(pre_es, "aim", [128, G]); ldt = sbt(pre_es, "ldt", [128, G])
            s.dma("sp", are[:], I["are2"], "spx", writes=["are"])
            s.dma("sp", aim[:], I["aim2"], "spx", writes=["aim"])
            s.dma("sp", ldt[:], I["ldt2"], "spx", writes=["ldt"])
            bx1 = sbt(pre_es, "bx1", [128, G, H]); bx2 = sbt(pre_es, "bx2", [128, G, H])
            ct1 = sbt(pre_es, "ct1", [128, G, H]); ct2 = sbt(pre_es, "ct2", [128, G, H])
            s.dma("sp", bx1[:], I["bx1"], "spx", writes=["bx1"])
            s.dma("sp", bx2[:], I["bx2"], "spx", writes=["bx2"])
            s.dma("sp", ct1[:], I["ct1"], "spx", writes=["ct1"])
            s.dma("sp", ct2[:], I["ct2"], "spx", writes=["ct2"])
            rowmask = sbt(pre_es, "rowmask", [128, 8])
            pass
            pass
            s.dma("sp", swp[:], I["c_swp"], "spx", writes=["swp"])
            s.dma("sp", rowmask[:], I["c_rowmask"], "spx", writes=["rowmask"])
            s.dma("sp", jidx[:], I["c_jidx"], "spx", writes=["jidx"])
            s.dma("sp", m01[:], I["c_m01"], "spx", writes=["m01"])
            s.dma("sp", h0s[:], I["h0s"], "spx", writes=["h0s"])
            s.commit_group(["are", "aim", "ldt", "bx1", "bx2", "ct1", "ct2", "swp", "rowmask", "jidx", "m01", "h0s"], "spx")
            PR = "ssmpar"
            dt_ = sbt(pre_es, "dt_", [128, G])
            t1 = sbt(pre_es, "t1", [128, G]); t2 = sbt(pre_es, "t2", [128, G]); ki = sbt(pre_es, "ki", [128, G], I32)
            cs = sbt(pre_es, "cs", [128, G]); sn = sbt(pre_es, "sn", [128, G])
            abr = sbt(pre_es, "abr", [128, G]); abi = sbt(pre_es, "abi", [128, G]); den = sbt(pre_es, "den", [128, G])
            cfr = sbt(pre_es, "cfr", [128, G]); cfi = sbt(pre_es, "cfi", [128, G]); cfin = sbt(pre_es, "cfin", [128, G])

            def dv(fn, rd=(), wr=()):
                s.op("dve", fn, reads=[PR] + list(rd), writes=[PR] + list(wr))

            def ac(fn, rd=(), wr=()):
                s.op("act", fn, reads=[PR] + list(rd), writes=[PR] + list(wr))

            dv(lambda e: e.tensor_scalar(are[:], are[:], -1e-4, None, ALU.min), rd=["are"])
            ac(lambda e: e.activation(dt_[:], ldt[:], AF.Exp), rd=["ldt"])
            dv(lambda e: e.tensor_tensor(t1[:], are[:], dt_[:], ALU.mult))
            ac(lambda e: e.activation(mag[:], t1[:], AF.Exp))
            dv(lambda e: e.tensor_tensor(th[:], aim[:], dt_[:], ALU.mult), rd=["aim"])
            dv(lambda e: e.tensor_scalar(t1[:], th[:], 1.0 / TWO_PI, None, ALU.mult))
            dv(lambda e: e.tensor_copy(ki[:], t1[:]))
            dv(lambda e: e.tensor_copy(t1[:], ki[:]))
            dv(lambda e: e.scalar_tensor_tensor(t2[:], t1[:], -CW1, th[:], ALU.mult, ALU.add))
            dv(lambda e: e.scalar_tensor_tensor(t2[:], t1[:], -CW2, t2[:], ALU.mult, ALU.add))
            dv(lambda e: e.tensor_scalar(t2[:], t2[:], math.pi, -math.pi, ALU.min, ALU.max))
            ac(lambda e: e.activation(sn[:], t2[:], AF.Sin))
            ac(lambda e: e.activation(t2[:], t2[:], AF.Abs))
            ac(lambda e: e.activation(cs[:], t2[:], AF.Sin, scale=-1.0, bias=halfpi[:, :]), rd=["halfpi"])
            dv(lambda e: e.tensor_tensor(abr[:], mag[:], cs[:], ALU.mult))
            dv(lambda e: e.tensor_tensor(abi[:], mag[:], sn[:], ALU.mult))
            dv(lambda e: e.tensor_tensor(den[:], are[:], are[:], ALU.mult))
            dv(lambda e: e.tensor_tensor(t1[:], aim[:], aim[:], ALU.mult))
            dv(lambda e: e.tensor_tensor(den[:], den[:], t1[:], ALU.add))
            dv(lambda e: e.reciprocal(den[:], den[:]))
            dv(lambda e: e.tensor_scalar(t1[:], abr[:], -1.0, None, ALU.add))
            dv(lambda e: e.tensor_tensor(cfr[:], t1[:], are[:], ALU.mult))
            dv(lambda e: e.tensor_tensor(t2[:], abi[:], aim[:], ALU.mult))
            dv(lambda e: e.tensor_tensor(cfr[:], cfr[:], t2[:], ALU.add))
            dv(lambda e: e.tensor_tensor(cfr[:], cfr[:], den[:], ALU.mult))
            dv(lambda e: e.tensor_tensor(cfi[:], abi[:], are[:], ALU.mult))
            dv(lambda e: e.tensor_tensor(t2[:], t1[:], aim[:], ALU.mult))
            dv(lambda e: e.tensor_tensor(cfi[:], cfi[:], t2[:], ALU.subtract))
            dv(lambda e: e.tensor_tensor(cfi[:], cfi[:], den[:], ALU.mult))
            dv(lambda e: e.tensor_scalar(cfin[:], cfi[:], sgn[:, 0:1], -1.0, ALU.mult, ALU.mult), rd=["sgn"])
            bbs = sbt(pre_es, "bbs", [128, G, H]); bbw = sbt(pre_es, "bbw", [128, G, H]); tb = sbt(pre_es, "tb", [128, G, H])
            cfr_b = cfr[:].unsqueeze(2).to_broadcast([128, G, H])
            cfin_b = cfin[:].unsqueeze(2).to_broadcast([128, G, H])
            dv(lambda e: e.tensor_tensor(bbs[:], bx1[:], cfr_b, ALU.mult), rd=["bx1"])
            dv(lambda e: e.tensor_tensor(tb[:], bx2[:], cfin_b, ALU.mult), rd=["bx2"])
            dv(lambda e: e.tensor_tensor(bbs[:], bbs[:], tb[:], ALU.add))
            dv(lambda e: e.tensor_tensor(bbw[:], bx2[:], cfr_b, ALU.mult))
            dv(lambda e: e.tensor_tensor(tb[:], bx1[:], cfin_b, ALU.mult))
            dv(lambda e: e.tensor_tensor(bbw[:], bbw[:], tb[:], ALU.subtract))
            pass
            tpc = pst(pre_es, "tpc", [128, 2, 128])
            for gc in range(4):
                s.op("pe", lambda e: e.transpose(tpc[:, 0, :], bbs[:, gc * 8:(gc + 1) * 8, :].rearrange("p g i -> p (g i)"), ident[:]),
                     reads=[PR, "ident"], writes=["tpc"])
                s.op("pe", lambda e: e.transpose(tpc[:, 1, :], bbw[:, gc * 8:(gc + 1) * 8, :].rearrange("p g i -> p (g i)"), ident[:]),
                     reads=[PR, "ident"], writes=["tpc"])
                for g8 in range(8):
                    g = gc * 8 + g8
                    s.op("dve", lambda e: e.tensor_scalar(BBp[:, g, :], tpc[:, 0, :], rowmask[:, g8:g8 + 1], None, ALU.mult),
                         reads=["tpc", "rowmask"], writes=["BBp"])
                    s.op("dve", lambda e: e.tensor_scalar(BBq[:, g, :], tpc[:, 1, :], rowmask[:, g8:g8 + 1], None, ALU.mult),
                         reads=["tpc", "rowmask"], writes=["BBq"])
            pass
            s.op("pool", lambda e: e.memset(CAp[:], 0.0), writes=["CAp"])
            s.op("pool", lambda e: e.memset(CBp[:], 0.0), writes=["CBp"])
            for g8 in range(8):
                s.op("dve", lambda e: e.tensor_scalar(CAp[:, g8::8, g8 * 16:(g8 + 1) * 16], ct1[:, g8::8, :], sgn[:, 0:1], None, ALU.mult),
                     reads=["ct1", "sgn", "CAp"], writes=["CAp"])
                s.op("dve", lambda e: e.tensor_scalar(CBp[:, g8::8, g8 * 16:(g8 + 1) * 16], ct2[:, g8::8, :], -1.0, None, ALU.mult),
                     reads=["ct2", "CBp"], writes=["CBp"])
            pass
            for m in range(4):
                s.op("dve", lambda e: e.tensor_scalar(dgD[:, m, :], ident[:], vec4[:, V_D, m:m + 1], None, ALU.mult),
                     reads=["ident", "vec4"], writes=["dgD"])

            s.barrier()
            pre_es.close()
            NI = 3
            NB = 2 * NI
            Yp = [pst(es, "Yp%d" % i, [128, 512]) for i in range(5)]
            Pp = [pst(es, "Pp%d" % i, [128, 512]) for i in range(2)]
            Cp = pst(es, "Cp", [128, 512])
            ang = [sbt(es, "ang%d" % i, [128, 576]) for i in range(NI)]
            kf = [sbt(es, "kf%d" % i, [128, 576]) for i in range(NB)]
            kint = sbt(es, "kint", [128, 576], I32)
            ctab = [sbt(es, "ctab%d" % i, [128, 576]) for i in range(NB)]
            stab = [sbt(es, "stab%d" % i, [128, 576]) for i in range(NB)]
            rotP = [sbt(es, "rotP%d" % i, [128, 128]) for i in range(NB)]
            rotS = [sbt(es, "rotS%d" % i, [128, 128]) for i in range(NB)]
            magS = [sbt(es, "magS%d" % i, [128, 64]) for i in range(NB)]
            NW_ = NI
            p1s = [sbt(es, "p1s%d" % i, [128, 512]) for i in range(NW_)]
            p2s = [sbt(es, "p2s%d" % i, [128, 512]) for i in range(NW_)]
            bp = [sbt(es, "bp%d" % i, [128, 512]) for i in range(NW_)]
            zz = [sbt(es, "zz%d" % i, [128, 512]) for i in range(NW_)]
            zc = [sbt(es, "zc%d" % i, [128, 512], BF16) for i in range(NW_)]
            zs = [sbt(es, "zs%d" % i, [128, 512], BF16) for i in range(NW_)]
            zc_f = sbt(es, "zcf", [128, 512])

            def build_tables_multi(gs):
                for x, g in enumerate(gs):
                    s.op("act", lambda e: e.activation(ang[x][:], jidx[:], AF.Copy, scale=th[:, g:g + 1]), reads=["jidx", PR], writes=["ang%d" % x])
                for x, g in enumerate(gs):
                    A = "ang%d" % x
                    s.op("dve", lambda e: e.tensor_scalar(kint[:], ang[x][:], 1.0 / TWO_PI, None, ALU.mult), reads=[A], writes=["kint"])
                    s.op("dve", lambda e: e.scalar_tensor_tensor(ang[x][:], kint[:], -CW1, ang[x][:], ALU.mult, ALU.add), reads=["kint", A], writes=[A])
                    s.op("dve", lambda e: e.scalar_tensor_tensor(ang[x][:], kint[:], -CW2, ang[x][:], ALU.mult, ALU.add), reads=["kint", A], writes=[A])
                    s.op("dve", lambda e: e.tensor_scalar(ang[x][:], ang[x][:], math.pi, -math.pi, ALU.min, ALU.max), reads=[A], writes=[A])
                for x, g in enumerate(gs):
                    q = g % NB
                    T = "tab%d" % q
                    A = "ang%d" % x
                    s.op("act", lambda e: e.activation(stab[q][:], ang[x][:], AF.Sin, scale=sgn[:, 0:1]), reads=[A, "sgn"], writes=[T])
                    s.op("act", lambda e: e.activation(kf[q][:], ang[x][:], AF.Sin), reads=[A], writes=["kf%d" % q])
                    s.op("act", lambda e: e.activation(ang[x][:], ang[x][:], AF.Abs), reads=[A, T], writes=[A])
                    s.op("act", lambda e: e.activation(ctab[q][:], ang[x][:], AF.Sin, scale=-1.0, bias=halfpi[:, :]), reads=[A, "halfpi"], writes=[T])

            def build_rots(g):
                q = g % NB
                T = "tab%d" % q
                for (rot, rn, ji) in ((rotP[q], "rotP%d" % q, 511), (rotS[q], "rotS%d" % q, 3)):
                    s.op("dve", lambda e: e.tensor_scalar(rot[:], ident[:], ctab[q][:, ji:ji + 1], None, ALU.mult), reads=["ident", T], writes=[rn])
                    s.op("dve", lambda e: e.scalar_tensor_tensor(rot[:], swp[:], stab[q][:, ji:ji + 1], rot[:], ALU.mult, ALU.add), reads=["swp", T, rn], writes=[rn])
                s.op("dve", lambda e: e.tensor_scalar(magS[q][:], m01[:], mag[:, g:g + 1], None, ALU.mult), reads=["m01", PR], writes=["magS%d" % q])

            def step_front(g, gc, bi, b0, bw, w):
                q = g % NB
                T = "tab%d" % q
                samp = (b0 >= SEQ)
                to = 512 if samp else 0
                s.op("pe", lambda e: e.matmul(Pp[0][:, 0:bw], BBp[:, g, :], uT[:, gc, b0:b0 + bw], start=True, stop=True),
                     reads=["BBp", "uT"], writes=["Pp0"])
                s.op("pe", lambda e: e.matmul(Pp[1][:, 0:bw], BBq[:, g, :], uT[:, gc, b0:b0 + bw], start=True, stop=True),
                     reads=["BBq", "uT"], writes=["Pp1"])
                s.op("act", lambda e: e.copy(p1s[w][:, 0:bw], Pp[0][:, 0:bw]), reads=["Pp0"], writes=["p1s%d" % w])
                s.op("act", lambda e: e.copy(p2s[w][:, 0:bw], Pp[1][:, 0:bw]), reads=["Pp1"], writes=["p2s%d" % w])
                s.op("dve", lambda e: e.tensor_tensor(bp[w][:, 0:bw], p1s[w][:, 0:bw], ctab[q][:, to:to + bw], ALU.mult),
                     reads=["p1s%d" % w, T], writes=["bp%d" % w])
                s.op("dve", lambda e: e.tensor_tensor(p2s[w][:, 0:bw], p2s[w][:, 0:bw], stab[q][:, to:to + bw], ALU.mult),
                     reads=["p2s%d" % w, T], writes=["p2s%d" % w])

            def step_scan(g, gc, bi, b0, bw, w):
                q = g % NB
                samp = (b0 >= SEQ)
                s.op("dve", lambda e: e.tensor_tensor(bp[w][:, 0:bw], bp[w][:, 0:bw], p2s[w][:, 0:bw], ALU.add),
                     reads=["bp%d" % w, "p2s%d" % w], writes=["bp%d" % w])
                if samp:
                    bpv = bp[w][:, 0:bw].rearrange("p (b t) -> p b t", t=TS)[:, :, 0]
                    s.op("dve", lambda e: e.scalar_tensor_tensor(bpv, h0s[:, g, :], mag[:, g:g + 1], bpv, ALU.mult, ALU.add),
                         reads=["h0s", PR, "bp%d" % w], writes=["bp%d" % w])
                    s.op("dve", lambda e: e.tensor_tensor_scan(zz[w][:, 0:bw], magS[q][:, 0:bw], bp[w][:, 0:bw], 0.0, ALU.mult, ALU.add),
                         reads=["magS%d" % q, "bp%d" % w], writes=["zz%d" % w])
                    s.op("pe", lambda e: e.matmul(Cp[:, 128:128 + NS], rotS[q][:], zz[w][:, 0:bw].rearrange("p (b t) -> p b t", t=TS)[:, :, TS - 1], start=True, stop=True),
                         reads=["rotS%d" % q, "zz%d" % w], writes=["Cp"])
                    s.op("act", lambda e: e.copy(stS[:, g, :], Cp[:, 128:128 + NS]), reads=["Cp"], writes=["stS"])
                else:
                    init = 0.0 if bi == 0 else hcar[:, bi - 1, g:g + 1]
                    s.op("dve", lambda e: e.tensor_tensor_scan(zz[w][:, 0:bw], mag[:, g:g + 1].to_broadcast([128, bw]), bp[w][:, 0:bw], init, ALU.mult, ALU.add),
                         reads=[PR, "bp%d" % w, "hcar%d" % g], writes=["zz%d" % w])
                    s.op("pe", lambda e: e.matmul(Cp[:, 160 + bi:161 + bi], rotP[q][:], zz[w][:, bw - 1:bw], start=True, stop=True),
                         reads=["rotP%d" % q, "zz%d" % w], writes=["Cp"])
                    s.op("act", lambda e: e.copy(hcar[:, bi, g:g + 1], Cp[:, 160 + bi:161 + bi]), reads=["Cp"], writes=["hcar%d" % g])

            def step_back(g, gc, bi, b0, bw, w, last):
                q = g % NB
                T = "tab%d" % q
                samp = (b0 >= SEQ)
                to = 512 if samp else 0
                s.op("dve", lambda e: e.tensor_tensor(zc[w][:, 0:bw], zz[w][:, 0:bw], ctab[q][:, to:to + bw], ALU.mult),
                     reads=["zz%d" % w, T], writes=["zc%d" % w])
                s.op("dve", lambda e: e.tensor_tensor(zs[w][:, 0:bw], zz[w][:, 0:bw], kf[q][:, to:to + bw], ALU.mult),
                     reads=["zz%d" % w, "kf%d" % q], writes=["zs%d" % w])
                s.op("pe", lambda e: e.matmul(Yp[bi][:, 0:bw], CAp[:, g, :], zc[w][:, 0:bw], start=False, stop=False),
                     reads=["CAp", "zc%d" % w], writes=["Yp%d" % bi], signal=False)
                s.op("pe", lambda e: e.matmul(Yp[bi][:, 0:bw], CBp[:, g, :], zs[w][:, 0:bw], start=False, stop=last),
                     reads=["CBp", "zs%d" % w], writes=["Yp%d" % bi], signal=True)

            build_tables_multi(list(range(min(NI, G))))
            for gi_ in range(min(NI, G)):
                build_rots(gi_)
            for gc in range(4):
                for bi, (b0, bw) in enumerate(BLKS):
                    s.op("pe", lambda e: e.matmul(Yp[bi][:, 0:bw], dgD[:, gc, :], uT[:, gc, b0:b0 + bw], start=True, stop=False),
                         reads=["dgD", "uT"], writes=["Yp%d" % bi], signal=False)
                for gq in range(0, 8, NI):
                    grp = [gc * 8 + gq + x for x in range(NI) if gq + x < 8]
                    build_tables_multi([g + NI for g in grp if g + NI < G])
                    for bi, (b0, bw) in enumerate(BLKS):
                        ws = [x for x in range(len(grp))]
                        if bi == 1:
                            for g in grp:
                                if g + NI < G:
                                    build_rots(g + NI)
                        for x, g in enumerate(grp):
                            step_front(g, gc, bi, b0, bw, ws[x])
                        for x, g in enumerate(grp):
                            step_scan(g, gc, bi, b0, bw, ws[x])
                        for x, g in enumerate(grp):
                            step_back(g, gc, bi, b0, bw, ws[x], last=(gq + x == 7))
                for bi, (b0, bw) in enumerate(BLKS):
                    yw = (gc * 5 + bi) % 2
                    s.op("act", lambda e: e.activation(ysSt[yw][:, 0:bw], Yp[bi][:, 0:bw], AF.Gelu_apprx_tanh),
                         reads=["Yp%d" % bi], writes=["ysSt%d" % yw])
                    s.op("dve", lambda e: e.tensor_copy(ysB[:, gc, b0:b0 + bw], ysSt[yw][:, 0:bw]), reads=["ysSt%d" % yw], writes=["ysB"])
                    s.dma("sp", ys_f_scr[:, gc, b0:b0 + bw], ysSt[yw][:, 0:bw], "ysfst%d" % yw, reads=["ysSt%d" % yw], writes=["ysf_%d_%d" % (gc, bi)])
            s.dma("sp", O["st_p"], hcar[:, 3, :], "ostp", reads=["hcar%d" % g_ for g_ in range(G)], writes=["o_st_p"])
            s.dma("sp", O["st_s"], stS[:], "osts", reads=["stS"], writes=["o_st_s"])
            s.dma("pool", wgl[:], I["w_glu"].rearrange("(k p) n -> p k n", p=128), "wgl", writes=["wgl"])
            sgg = [p1s[0], p1s[1], p2s[0], p2s[1]]
            sqq = [bp[0], bp[1], bp[2], p1s[2]]
            SGN = ["p1s0", "p1s1", "p2s0", "p2s1"]
            SQN = ["bp0", "bp1", "bp2", "p1s2"]
            rs3 = ang[0]
            ysFb = [zz[0], zz[1], zz[2], zc_f]
            YFN = ["zz0", "zz1", "zz2", "zcf"]
            for bi, (b0, bw) in enumerate(BLKS):
                for m in range(4):
                    s.dma("sp", ysFb[m][:, 0:bw], ys_f_scr[:, m, b0:b0 + bw], "ysfld%d" % m, reads=["ysf_%d_%d" % (m, bi)], writes=[YFN[m]])
                for m in range(4):
                    for k in range(4):
                        s.op("pe", lambda e: e.matmul(Yp[m][:, 0:bw], wgl[:, k, m * 128:(m + 1) * 128], ysB[:, k, b0:b0 + bw], start=(k == 0), stop=(k == 3)),
                             reads=["wgl", "ysB"], writes=["Yp%d" % m], signal=(k == 3))
                    s.op("act", lambda e: e.activation(sgg[m][:, 0:bw], Yp[m][:, 0:bw], AF.Sigmoid, bias=vec4[:, V_BGLU, m:m + 1]),
                         reads=["Yp%d" % m, "vec4"], writes=[SGN[m]])
                    s.op("dve", lambda e: e.tensor_tensor(sgg[m][:, 0:bw], sgg[m][:, 0:bw], ysFb[m][:, 0:bw], ALU.mult),
                         reads=[SGN[m], YFN[m]], writes=[SGN[m]])
                    s.op("pool", lambda e: e.tensor_tensor(sqq[m][:, 0:bw], sgg[m][:, 0:bw], sgg[m][:, 0:bw], ALU.mult),
                         reads=[SGN[m]], writes=[SQN[m]])
                for m in range(4):
                    s.op("pe", lambda e: e.matmul(Pp[0][:, 0:bw], ones32[:], sqq[m][:, 0:bw], start=(m == 0), stop=(m == 3)),
                         reads=["ones32", SQN[m]], writes=["Pp0"], signal=(m == 3))
                s.op("act", lambda e: e.activation(rs3[:, 0:bw], Pp[0][:, 0:bw], AF.Sqrt, scale=1.0 / 512, bias=epsc[:, :]),
                     reads=["Pp0", "epsc"], writes=["ang0"])
                s.op("dve", lambda e: e.reciprocal(rs3[:, 0:bw], rs3[:, 0:bw]), reads=["ang0"], writes=["ang0"])
                for m in range(4):
                    s.op("dve", lambda e: e.scalar_tensor_tensor(mergedT[:, m, b0:b0 + bw], sgg[m][:, 0:bw], vec4[:, V_GOS, m:m + 1], rs3[:, 0:bw], ALU.mult, ALU.mult),
                         reads=[SGN[m], "vec4", "ang0"], writes=["mergedT"])
            if dbg:
                d_m = dbg_out("mergedT", [128, 8, NTOK])
                dtmp2 = sbt(es, "dtmp2", [128, 8, NTOK])
                s.op("dve", lambda e: e.tensor_copy(dtmp2[:], mergedT[:]), reads=["mergedT"], writes=["dtmp2"])
                s.dma("sp", d_m, dtmp2[:], "dbg1", reads=["dtmp2"], writes=["d_m"])

        late = ExitStack()
        glob.enter_context(late)
        s.op("pool", lambda e: e.memset(logit[:], 0.0), writes=["logit"])
        with phase() as es:
            md = load_mod(es, [2, 3, 4], ["gt1", "sh2", "sc2"])
            gffn = sbt(es, "gffn", [128, D])
            s.dma("sp", gffn[:], I["g_ffn"].to_broadcast([128, D]), "gf", writes=["gffn"])
            for (tl, nm, R) in ((md["sc2"][0], "modP_sc2", 128), (md["sc2"][1], "modS_sc2", 64)):
                s.op("dve", lambda e: e.scalar_tensor_tensor(tl[0:R, :], tl[0:R, :], 1.0, gffn[0:R, :], ALU.add, ALU.mult),
                     reads=[nm, "gffn"], writes=[nm])
            w_out_b = sbt(es, "w_out_b", [128, 8, D], BF16)
            s.dma("pool", w_out_b[:], I["w_out"].rearrange("(k p) n -> p k n", p=128), "wout", writes=["w_out_b"])
            wrt = sbt(es, "wrt", [128, 8, 36])
            s.dma("sp", wrt[:], I["w_rt"].rearrange("(k p) n -> p k n", p=128), "wrt", writes=["wrt"])
            brt = sbt(es, "brt", [128, 36])
            s.dma("sp", brt[:], I["b_rt"].to_broadcast([128, 36]), "brt", writes=["brt"])
            xt = [sbt(es, "xtD%d" % i, [128, D]) for i in range(3)]
            x1 = [sbt(es, "x1D%d" % i, [128, D]) for i in range(3)]
            n2 = [sbt(es, "n2D%d" % i, [128, D]) for i in range(3)]
            n2Tf = [sbt(es, "n2Tf%d" % i, [128, 8, 128]) for i in range(3)]
            n2Tb = [sbt(es, "n2Tb%d" % i, [128, D], BF16) for i in range(3)]
            junk = sbt(es, "junkD", [128, D])
            ssq = sbt(es, "ssqD", [128, NT]); rstd = sbt(es, "rstdD", [128, NT])
            mo = [pst(es, "moD%d" % i, [128, 512]) for i in range(4)]
            tpp = [pst(es, "tpD%d" % i, [128, 8, 128]) for i in range(1)]
            lp = pst(es, "lpD", [128, 36])
            def stage_a(i):
                R = tile_rows(i)
                c0 = i * 128
                w = i % 3
                pi_ = 0 if i < 16 else 1
                xb, xn = xt[w], "xtD%d" % w
                s.dma("sp", xb[0:R, :], I["x_all"][c0:c0 + R, :], xn, writes=[xn])
                for h in range(2):
                    mb, mn = mo[(i % 2) * 2 + h], "moD%d" % ((i % 2) * 2 + h)
                    for k in range(8):
                        s.op("pe", lambda e: e.matmul(mb[0:R, :], mergedT[:, k, c0:c0 + R], w_out_b[:, k, h * 512:(h + 1) * 512], start=(k == 0), stop=(k == 7)),
                             reads=["mergedT", "w_out_b"], writes=[mn], signal=(k == 7))
                    pfx = "modP_" if i < 16 else "modS_"
                    s.op("dve", lambda e: e.tensor_tensor(x1[w][0:R, h * 512:(h + 1) * 512], mb[0:R, :], md["gt1"][pi_][0:R, h * 512:(h + 1) * 512], ALU.mult),
                         reads=[mn, pfx + "gt1"], writes=["x1D%d" % w])
                s.op("dve", lambda e: e.tensor_tensor(x1[w][0:R, :], x1[w][0:R, :], xb[0:R, :], ALU.add), reads=["x1D%d" % w, xn], writes=["x1D%d" % w])
                s.dma("sp", x1_scr[c0:c0 + R, :], x1[w][0:R, :], "x1st%d" % w, reads=["x1D%d" % w], writes=["x1_scr%d" % i])
                s.op("act", lambda e: e.activation(junk[0:R, :], x1[w][0:R, :], AF.Square, accum_out=ssq[0:R, i:i + 1]),
                     reads=["x1D%d" % w], writes=["junkD", "ssqD"])
                s.op("act", lambda e: e.activation(rstd[0:R, i:i + 1], ssq[0:R, i:i + 1], AF.Sqrt, scale=1.0 / D, bias=epsc[0:R, :]),
                     reads=["ssqD", "epsc"], writes=["rstdD"])
                s.op("dve", lambda e: e.reciprocal(rstd[0:R, i:i + 1], rstd[0:R, i:i + 1]), reads=["rstdD"], writes=["rstdD"])
                s.op("dve", lambda e: e.scalar_tensor_tensor(n2[w][0:R, :], x1[w][0:R, :], rstd[0:R, i:i + 1], md["sc2"][pi_][0:R, :], ALU.mult, ALU.mult),
                     reads=["x1D%d" % w, "rstdD", pfx + "sc2"], writes=["n2D%d" % w])
                s.op("dve", lambda e: e.tensor_tensor(n2[w][0:R, :], n2[w][0:R, :], md["sh2"][pi_][0:R, :], ALU.add),
                     reads=["n2D%d" % w, pfx + "sh2"], writes=["n2D%d" % w])

            def stage_b(i):
                R = tile_rows(i)
                c0 = i * 128
                w = i % 3
                tpb = tpp[0]
                for k in range(8):
                    s.op("pe", lambda e: e.transpose(tpb[:, k, 0:R], n2[w][0:R, k * 128:(k + 1) * 128], ident[0:R, 0:R]),
                         reads=["n2D%d" % w, "ident"], writes=["tpD0"], signal=(k == 7))
                s.op("act", lambda e: e.copy(n2Tb[w][0:R, :], n2[w][0:R, :]), reads=["n2D%d" % w], writes=["n2Tb%d" % w])
                s.dma("sp", n2_scr[c0:c0 + R, :], n2Tb[w][0:R, :], "n2st%d" % w, reads=["n2Tb%d" % w], writes=["n2_scr%d" % i])
                s.op("dve", lambda e: e.tensor_copy(n2Tf[w][:, :, 0:R], tpb[:, :, 0:R]), reads=["tpD0"], writes=["n2Tf%d" % w])

            def stage_c(i):
                R = tile_rows(i)
                w = i % 3
                for k in range(8):
                    s.op("pe", lambda e: e.matmul(lp[0:R, :], n2Tf[w][:, k, 0:R], wrt[:, k, :], start=(k == 0), stop=(k == 7)),
                         reads=["n2Tf%d" % w, "wrt"], writes=["lpD"], signal=(k == 7))
                s.op("dve", lambda e: e.tensor_tensor(logit[0:R, i, :], lp[0:R, :], brt[0:R, :], ALU.add), reads=["lpD", "brt"], writes=["logit"])


            for i in range(NT + 2):
                if i < NT:
                    stage_a(i)
                if 1 <= i <= NT:
                    stage_b(i - 1)
                if i >= 2:
                    stage_c(i - 2)
        s.barrier()
        mix_es.close()
        slot_i = sbt(late, "slot_i", [128, 2, NT], I32)
        wk = sbt(late, "wk", [128, 2, NT])
        NW = 3
        w1b = [sbt(late, "w1b%d" % i, [128, 8, DE], BF16) for i in range(NW)]
        w3b = [sbt(late, "w3b%d" % i, [128, 8, DE], BF16) for i in range(NW)]
        w2b = [sbt(late, "w2b%d" % i, [128, 4, D], BF16) for i in range(NW)]

        def load_w(e_):
            w = e_ % NW
            s.dma("pool", w1b[w][:], I["w1"][e_].rearrange("(k p) n -> p k n", p=128), "w1b%d" % w, writes=["w1b%d" % w])
            s.dma("pool", w3b[w][:], I["w3"][e_].rearrange("(k p) n -> p k n", p=128), "w3b%d" % w, writes=["w3b%d" % w])
            s.dma("pool", w2b[w][:], I["w2"][e_].rearrange("(k p) n -> p k n", p=128), "w2b%d" % w, writes=["w2b%d" % w])

        for e_ in range(min(NW - 1, n_exp)):
            load_w(e_)
        with phase() as es:
            RT = "route"
            A3 = [128, NT]
            mx = sbt(es, "mx", A3); sm = sbt(es, "sm", A3); ptop = sbt(es, "ptop", A3)
            eg = sbt(es, "eg", [128, NT, 4]); ohg = sbt(es, "ohg", [128, NT, 4])
            sel = sbt(es, "sel", [128, NT, 8]); tmp8 = sbt(es, "tmp8", [128, NT, 8])
            m1 = sbt(es, "m1", A3); m2 = sbt(es, "m2", A3)
            k1 = sbt(es, "k1", [128, NT, 8]); k2 = sbt(es, "k2", [128, NT, 8])
            oh = [sbt(es, "oh%d" % i, [128, NT, 32]) for i in range(2)]
            mk = sbt(es, "mk", [128, NT, 32]); mkb = sbt(es, "mkb", [128, NT * 32], BF16)
            pos = sbt(es, "pos", [128, NT, 32]); offs = sbt(es, "offs", [128, NT, 32])
            t32 = sbt(es, "t32", [128, NT, 32])
            sl = sbt(es, "sl", [128, 2, NT]); pk = sbt(es, "pk", [128, 2, NT])
            ltri = sbt(es, "ltri", [128, 128]); ltrib = sbt(es, "ltrib", [128, 128], BF16)
            onesb = sbt(es, "onesb", [128, 128], BF16)
            eoff = sbt(es, "eoff", [128, 32])
            cup = pst(es, "cup", [128, 2, 512])
            top = pst(es, "top", [128, 2, 512])
            s.dma("sp", ltri[:], I["c_ltri"], "e0", writes=["ltri"])
            s.dma("sp", eoff[:], I["c_eoff"], "e1", writes=["eoff"])
            s.op("dve", lambda e: e.tensor_copy(ltrib[:], ltri[:]), reads=["ltri"], writes=["ltrib"])
            s.op("dve", lambda e: e.tensor_copy(onesb[:], ones32[:]), reads=["ones32"], writes=["onesb"])

            def rv(fn, rd=(), wr=()):
                s.op("dve", fn, reads=[RT] + list(rd), writes=[RT] + list(wr))

            def ra(fn):
                s.op("act", fn, reads=[RT], writes=[RT])

            def bc(ap2, n):
                return ap2.unsqueeze(2).to_broadcast([128, NT, n])

            lg = logit[:, :, 0:4]
            rv(lambda e: e.tensor_reduce(mx[:], lg, AX.X, ALU.max), rd=["logit"])
            rv(lambda e: e.tensor_tensor(eg[:], lg, bc(mx[:], 4), ALU.subtract), rd=["logit"])
            rv(lambda e: e.tensor_single_scalar(ohg[:], eg[:], 0.0, ALU.is_ge))
            ra(lambda e: e.activation(eg[:], eg[:], AF.Exp))
            rv(lambda e: e.tensor_reduce(sm[:], eg[:], AX.X, ALU.add))
            rv(lambda e: e.reciprocal(ptop[:], sm[:]))
            for g_ in range(4):
                le = logit[:, :, 4 + 8 * g_:12 + 8 * g_]
                if g_ == 0:
                    rv(lambda e: e.tensor_tensor(sel[:], le, bc(ohg[:, :, 0], 8), ALU.mult), rd=["logit"])
                else:
                    rv(lambda e: e.tensor_tensor(tmp8[:], le, bc(ohg[:, :, g_], 8), ALU.mult), rd=["logit"])
                    rv(lambda e: e.tensor_tensor(sel[:], sel[:], tmp8[:], ALU.add))
            rv(lambda e: e.tensor_reduce(m1[:], sel[:], AX.X, ALU.max))
            rv(lambda e: e.tensor_tensor(k1[:], sel[:], bc(m1[:], 8), ALU.is_ge))
            rv(lambda e: e.scalar_tensor_tensor(tmp8[:], k1[:], -1e30, sel[:], ALU.mult, ALU.add))
            rv(lambda e: e.tensor_reduce(m2[:], tmp8[:], AX.X, ALU.max))
            rv(lambda e: e.tensor_tensor(k2[:], tmp8[:], bc(m2[:], 8), ALU.is_ge))
            rv(lambda e: e.tensor_tensor(wk[:, 0, :], m2[:], m1[:], ALU.subtract), wr=["wk"])
            s.op("act", lambda e: e.activation(wk[:, 0, :], wk[:, 0, :], AF.Exp), reads=[RT, "wk"], writes=[RT, "wk"])
            rv(lambda e: e.tensor_scalar(wk[:, 0, :], wk[:, 0, :], 1.0, None, ALU.add), wr=["wk"])
            rv(lambda e: e.reciprocal(wk[:, 0, :], wk[:, 0, :]), wr=["wk"])
            rv(lambda e: e.tensor_tensor(wk[:, 0, :], wk[:, 0, :], ptop[:], ALU.mult), wr=["wk"])
            rv(lambda e: e.tensor_tensor(wk[:, 1, :], ptop[:], wk[:, 0, :], ALU.subtract), wr=["wk"])
            for g_ in range(4):
                rv(lambda e: e.tensor_tensor(oh[0][:, :, 8 * g_:8 * g_ + 8], k1[:], bc(ohg[:, :, g_], 8), ALU.mult))
                rv(lambda e: e.tensor_tensor(oh[1][:, :, 8 * g_:8 * g_ + 8], k2[:], bc(ohg[:, :, g_], 8), ALU.mult))
            rv(lambda e: e.tensor_tensor(mk[:], oh[0][:], oh[1][:], ALU.add))
            rv(lambda e: e.memset(mk[64:128, NT - 1, :], 0.0))
            rv(lambda e: e.tensor_copy(mkb[:], mk[:].rearrange("p i e -> p (i e)")), wr=["mkb"])
            NH = NT * 32 // 2
            for h_ in range(2):
                s.op("pe", lambda e: e.matmul(cup[:, h_, 0:NH], ltrib[:], mkb[:, h_ * NH:(h_ + 1) * NH], start=True, stop=True),
                     reads=["ltrib", "mkb"], writes=["cup"])
                s.op("pe", lambda e: e.matmul(top[:, h_, 0:NH], onesb[:], mkb[:, h_ * NH:(h_ + 1) * NH], start=True, stop=True),
                     reads=["onesb", "mkb"], writes=["top"])
            posf = pos[:].rearrange("p i e -> p (i e)")
            t32f = t32[:].rearrange("p i e -> p (i e)")
            for h_ in range(2):
                rv(lambda e: e.tensor_copy(posf[:, h_ * NH:(h_ + 1) * NH], cup[:, h_, 0:NH]), rd=["cup"])
                rv(lambda e: e.tensor_copy(t32f[:, h_ * NH:(h_ + 1) * NH], top[:, h_, 0:NH]), rd=["top"])
            rv(lambda e: e.memset(offs[:, 0, :], 0.0))
            for i in range(1, NT):
                rv(lambda e: e.tensor_tensor(offs[:, i, :], offs[:, i - 1, :], t32[:, i - 1, :], ALU.add))
            rv(lambda e: e.tensor_tensor(pos[:], pos[:], offs[:], ALU.add))
            for k_ in range(2):
                rv(lambda e: e.tensor_tensor(t32[:], oh[k_][:], pos[:], ALU.mult))
                rv(lambda e: e.tensor_reduce(pk[:, k_, :], t32[:], AX.X, ALU.add))
                rv(lambda e: e.tensor_tensor(t32[:], oh[k_][:], eoff[:].unsqueeze(1).to_broadcast([128, NT, 32]), ALU.mult), rd=["eoff"])
                rv(lambda e: e.tensor_reduce(sl[:, k_, :], t32[:], AX.X, ALU.add))
            rv(lambda e: e.tensor_scalar(pk[:], pk[:], float(CAP - 1), None, ALU.min))
            rv(lambda e: e.tensor_tensor(sl[:], sl[:], pk[:], ALU.add))
            rv(lambda e: e.tensor_copy(slot_i[:], sl[:]), wr=["slot_i"])
            if dbg:
                d_c = dbg_out("slots", [128, 2, NT])
                s.dma("sp", d_c, sl[:], "dbg2", reads=[RT], writes=["d_c"])
            nb2 = [sbt(es, "nb2_%d" % i, [128, D], BF16) for i in range(4)]
            for i in range(NT):
                R = tile_rows(i)
                w = i % 4
                s.dma("sp", nb2[w][0:R, :], n2_scr[i * 128:i * 128 + R, :], "nb2ld%d" % w, reads=["n2_scr%d" % i], writes=["nb2_%d" % w])
                for k_ in range(2):
                    s.dma_custom("pool", lambda e: e.indirect_dma_start(
                        out=xs_scr[:, :], out_offset=bass.IndirectOffsetOnAxis(ap=slot_i[0:R, k_, i:i + 1], axis=0),
                        in_=nb2[w][0:R, :], in_offset=None),
                        "sc%d" % w, reads=["nb2_%d" % w, "slot_i", "xs_zf"], writes=["xs_scr_%d" % w])

        with phase() as es:
            xe = [sbt(es, "xe%d" % i, [128, NJ, D], BF16) for i in range(2)]
            xT = [sbt(es, "xT%d" % i, [128, 8, CAP], BF16) for i in range(2)]
            hid = [sbt(es, "hid%d" % i, [128, 4, CAP], BF16) for i in range(2)]
            sgs = [sbt(es, "sgs%d" % i, [128, CAP]) for i in range(2)]
            yo = [sbt(es, "yo%d" % i, [128, D]) for i in range(2)]
            gup = [pst(es, "gup%d" % i, [128, 512]) for i in range(4)]
            dnp = [pst(es, "dnp%d" % i, [128, 512]) for i in range(2)]
            tpx = [pst(es, "tpx%d" % i, [128, 8, 128], BF16) for i in range(2)]
            cnt_g = 0
            cnt_d = 0
            cnt_t = 0
            cnt_y = 0

            def load_x(e_):
                v = e_ % 2
                s.dma("sp", xe[v][:], xs_scr[e_ * CAP:(e_ + 1) * CAP, :].rearrange("(j p) d -> p j d", p=128), "xe%d" % v,
                      reads=["xs_scr_0", "xs_scr_1", "xs_zf"], writes=["xe%d" % v])

            def do_T(e_):
                nonlocal cnt_t
                v = e_ % 2
                for j in range(NJ):
                    tb_ = tpx[cnt_t % 2]
                    tn = "tpx%d" % (cnt_t % 2)
                    cnt_t += 1
                    for k in range(8):
                        s.op("pe", lambda e: e.transpose(tb_[:, k, :], xe[v][:, j, k * 128:(k + 1) * 128], identb[:]),
                             reads=["xe%d" % v, "identb"], writes=[tn], signal=(k == 7))
                    if j % 2 == 0:
                        s.op("act", lambda e: e.copy(xT[v][:, :, j * 128:(j + 1) * 128], tb_[:]), reads=[tn], writes=["xT%d" % v])
                    else:
                        s.op("dve", lambda e: e.tensor_copy(xT[v][:, :, j * 128:(j + 1) * 128], tb_[:]), reads=[tn], writes=["xT%d" % v])

            def do_GU(e_):
                nonlocal cnt_g
                w = e_ % NW
                v = e_ % 2
                for m in range(4):
                    gi = cnt_g % 2
                    cnt_g += 1
                    gp, gpn = gup[gi * 2], "gup%d" % (gi * 2)
                    up, upn = gup[gi * 2 + 1], "gup%d" % (gi * 2 + 1)
                    for k in range(8):
                        s.op("pe", lambda e: e.matmul(gp[:, 0:CAP], w1b[w][:, k, m * 128:(m + 1) * 128], xT[v][:, k, :], start=(k == 0), stop=(k == 7)),
                             reads=["w1b%d" % w, "xT%d" % v], writes=[gpn], signal=(k == 7))
                    for k in range(8):
                        s.op("pe", lambda e: e.matmul(up[:, 0:CAP], w3b[w][:, k, m * 128:(m + 1) * 128], xT[v][:, k, :], start=(k == 0), stop=(k == 7)),
                             reads=["w3b%d" % w, "xT%d" % v], writes=[upn], signal=(k == 7))
                    s.op("act", lambda e: e.activation(sgs[gi][:, :], gp[:, 0:CAP], AF.Silu), reads=[gpn], writes=["sgs%d" % gi])
                    s.op("dve", lambda e: e.tensor_tensor(hid[v][:, m, :], up[:, 0:CAP], sgs[gi][:, :], ALU.mult), reads=[upn, "sgs%d" % gi], writes=["hid%d" % v])

            def do_DN(e_):
                nonlocal cnt_d, cnt_y
                w = e_ % NW
                v = e_ % 2
                for j in range(NJ):
                    yv = cnt_y % 2
                    cnt_y += 1
                    for h in range(2):
                        di = cnt_d % 2
                        cnt_d += 1
                        for m in range(4):
                            s.op("pe", lambda e: e.matmul(dnp[di][:, :], hid[v][:, m, j * 128:(j + 1) * 128], w2b[w][:, m, h * 512:(h + 1) * 512], start=(m == 0), stop=(m == 3)),
                                 reads=["hid%d" % v, "w2b%d" % w], writes=["dnp%d" % di], signal=(m == 3))
                        if h == 0:
                            s.op("act", lambda e: e.copy(yo[yv][:, 0:512], dnp[di][:, :]), reads=["dnp%d" % di], writes=["yo%d" % yv])
                        else:
                            s.op("dve", lambda e: e.tensor_copy(yo[yv][:, 512:1024], dnp[di][:, :]), reads=["dnp%d" % di], writes=["yo%d" % yv])
                    r0 = e_ * CAP + j * 128
                    s.dma("sp", ys_scr[r0:r0 + 128, :], yo[yv][:], "ysst%d" % yv, reads=["yo%d" % yv], writes=["ys_scr_%d" % yv])

            load_x(0)
            if n_exp > 1:
                load_x(1)
            do_T(0)
            for e_ in range(n_exp):
                if e_ + NW - 1 < n_exp:
                    load_w(e_ + NW - 1)
                do_GU(e_)
                if e_ + 1 < n_exp:
                    do_T(e_ + 1)
                if e_ + 2 < n_exp:
                    load_x(e_ + 2)
                do_DN(e_)

        with phase() as es:
            md = load_mod(es, [5], ["gt2"])
            gfin = sbt(es, "gfin", [128, D])
            s.dma("sp", gfin[:], I["g_fin"].to_broadcast([128, D]), "gfi", writes=["gfin"])
            x1 = [sbt(es, "x1G%d" % i, [128, D]) for i in range(6)]
            g0 = [sbt(es, "g0G%d" % i, [128, D]) for i in range(6)]
            g1 = [sbt(es, "g1G%d" % i, [128, D]) for i in range(6)]
            yo = [sbt(es, "yoG%d" % i, [128, D]) for i in range(6)]
            junk = sbt(es, "junkG", [128, D])
            ssq = sbt(es, "ssqG", [128, NT]); rstd = sbt(es, "rstdG", [128, NT])
            for i in range(NT):
                R = tile_rows(i)
                c0 = i * 128
                w = i % 6
                pi_ = 0 if i < 16 else 1
                pfx = "modP_" if i < 16 else "modS_"
                s.dma("sp", x1[w][0:R, :], x1_scr[c0:c0 + R, :], "x1ld%d" % w, reads=["x1_scr%d" % i], writes=["x1G%d" % w])
                for (gt, gn, k_) in ((g0[w], "g0G%d" % w, 0), (g1[w], "g1G%d" % w, 1)):
                    s.dma_custom("pool", lambda e: e.indirect_dma_start(
                        out=gt[0:R, :], out_offset=None, in_=ys_scr[:, :],
                        in_offset=bass.IndirectOffsetOnAxis(ap=slot_i[0:R, k_, i:i + 1], axis=0)),
                        "gG%d" % w, reads=["ys_scr_0", "ys_scr_1", "slot_i"], writes=[gn])
                s.op("act", lambda e: e.activation(g0[w][0:R, :], g0[w][0:R, :], AF.Copy, scale=wk[0:R, 0, i:i + 1]),
                     reads=["g0G%d" % w, "g1G%d" % w, "wk"], writes=["g0G%d" % w])
                s.op("dve", lambda e: e.scalar_tensor_tensor(g0[w][0:R, :], g1[w][0:R, :], wk[0:R, 1, i:i + 1], g0[w][0:R, :], ALU.mult, ALU.add),
                     reads=["g1G%d" % w, "g0G%d" % w, "wk"], writes=["g0G%d" % w])
                s.op("dve", lambda e: e.tensor_tensor(yo[w][0:R, :], g0[w][0:R, :], md["gt2"][pi_][0:R, :], ALU.mult),
                     reads=["g0G%d" % w, pfx + "gt2"], writes=["yoG%d" % w])
                s.op("dve", lambda e: e.tensor_tensor(x1[w][0:R, :], x1[w][0:R, :], yo[w][0:R, :], ALU.add),
                     reads=["x1G%d" % w, "yoG%d" % w], writes=["x1G%d" % w])
                s.op("act", lambda e: e.activation(junk[0:R, :], x1[w][0:R, :], AF.Square, accum_out=ssq[0:R, i:i + 1]),
                     reads=["x1G%d" % w], writes=["junkG", "ssqG"])
                s.op("act", lambda e: e.activation(rstd[0:R, i:i + 1], ssq[0:R, i:i + 1], AF.Sqrt, scale=1.0 / D, bias=epsc[0:R, :]),
                     reads=["ssqG", "epsc"], writes=["rstdG"])
                s.op("dve", lambda e: e.reciprocal(rstd[0:R, i:i + 1], rstd[0:R, i:i + 1]), reads=["rstdG"], writes=["rstdG"])
                s.op("dve", lambda e: e.scalar_tensor_tensor(yo[w][0:R, :], x1[w][0:R, :], rstd[0:R, i:i + 1], gfin[0:R, :], ALU.mult, ALU.mult),
                     reads=["x1G%d" % w, "rstdG", "gfin"], writes=["yoG%d" % w])
                s.dma("sp", O["y_all"][c0:c0 + R, :], yo[w][0:R, :], "yst%d" % w, reads=["yoG%d" % w], writes=["o_y%d" % i])
        s.finish("sp")
    return nc, list(DBG.keys())


def _consts():
    ident = np.eye(128, dtype=np.float32)
    swp = np.zeros((128, 128), np.float32)
    for k in range(64):
        swp[k, k + 64] = 1.0
        swp[k + 64, k] = 1.0
    rowmask = np.zeros((128, 8), np.float32)
    for p in range(128):
        rowmask[p, p // 16] = 1.0
    sgn = np.ones((128, 1), np.float32)
    sgn[64:] = -1.0
    j = np.concatenate([np.arange(1, 513), np.tile(np.arange(1, TS + 1), NS)]).astype(np.float32)
    jidx = np.broadcast_to(j[None, :], (128, 576)).copy()
    m01 = np.ones((128, NS, TS), np.float32)
    m01[:, :, 0] = 0.0
    ltri = np.triu(np.ones((128, 128), np.float32), 1)
    eoff = np.broadcast_to((np.arange(32, dtype=np.float32) * CAP)[None, :], (128, 32)).copy()
    return dict(c_ident=ident, c_swp=swp, c_rowmask=rowmask, c_sgn=sgn, c_jidx=jidx, c_m01=m01.reshape(128, 64),
                c_ltri=ltri, c_eoff=eoff)


def make_in_maps(inp):
    f = lambda a: np.ascontiguousarray(np.asarray(a, dtype=np.float32))
    sh = {}
    sh["w_ada"] = f(inp["w_ada"][0]); sh["b_ada"] = f(inp["b_ada"][0][None, :])
    sh["g_mix"] = f(inp["g_norm_mix"][0][None, :]); sh["g_ffn"] = f(inp["g_norm_ffn"][0][None, :])
    sh["g_fin"] = f(inp["g_final"][None, :])
    sh["w_in"] = f(inp["w_in"][0]); sh["w_out"] = f(inp["w_out"][0]); sh["w_glu"] = f(inp["w_ssm_glu"][0])
    are = np.asarray(inp["ssm_a_re"][0]).T; aim = np.asarray(inp["ssm_a_im"][0]).T
    sh["are2"] = f(np.concatenate([are, are], 0)); sh["aim2"] = f(np.concatenate([aim, aim], 0))
    sh["ldt2"] = f(np.broadcast_to(np.asarray(inp["ssm_log_dt"][0])[None, :], (128, G)))
    br = np.asarray(inp["ssm_b_re"][0]).transpose(1, 0, 2); bi = np.asarray(inp["ssm_b_im"][0]).transpose(1, 0, 2)
    sh["bx1"] = f(np.concatenate([br, bi], 0)); sh["bx2"] = f(np.concatenate([bi, br], 0))
    cr = np.asarray(inp["ssm_c_re"][0]).transpose(2, 0, 1); ci = np.asarray(inp["ssm_c_im"][0]).transpose(2, 0, 1)
    sh["ct1"] = f(np.concatenate([cr, ci], 0)); sh["ct2"] = f(np.concatenate([ci, cr], 0))
    vecs = [np.asarray(inp["ssm_d"][0]).reshape(512), inp["b_ssm_glu"][0], inp["b_dw"][0], inp["ln_conv_g"][0],
            inp["ln_conv_b"][0], inp["g_out_ssm"][0], inp["g_out_conv"][0]]
    sh["vec4"] = f(np.stack([np.asarray(v).reshape(4, 128).T for v in vecs], 1))
    sh["wdwT"] = f(np.asarray(inp["w_dw"][0]).reshape(CW, 4, 128).transpose(2, 1, 0))
    sh["w_rt"] = f(np.concatenate([np.asarray(inp["w_router_grp"][0]), np.asarray(inp["w_router_exp"][0]).reshape(D, 32)], 1))
    sh["b_rt"] = f(np.concatenate([np.asarray(inp["b_router_grp"][0]), np.asarray(inp["b_router_exp"][0]).reshape(32)])[None, :])
    sh["w1"] = f(inp["w_exp_gate"][0]); sh["w3"] = f(inp["w_exp_up"][0]); sh["w2"] = f(inp["w_exp_down"][0])
    sh.update(_consts())
    xp = np.asarray(inp["x_prompt"]); xs = np.asarray(inp["x_sample"])
    cp = np.asarray(inp["c_prompt"]); cs = np.asarray(inp["c_sample"])
    sre = np.asarray(inp["state_ssm_re"][0]); sim = np.asarray(inp["state_ssm_im"][0])
    cc = np.asarray(inp["cache_conv"][0])
    maps = []
    for c in range(NCORES):
        m = dict(sh)
        bs = slice(c * NS, (c + 1) * NS)
        m["x_all"] = f(np.concatenate([xp[c], xs[bs].reshape(NS * TS, D)], 0))
        m["c_all"] = f(np.concatenate([cp[c:c + 1], cs[bs]], 0))
        m["h0s"] = f(np.concatenate([sre[bs].transpose(2, 1, 0), sim[bs].transpose(2, 1, 0)], 0))
        m["cachef"] = f(cc[bs].reshape(NS, CB, 4, 128).transpose(3, 2, 0, 1))
        maps.append(m)
    return maps


def assemble(results, inp):
    y_p = np.zeros((8, SEQ, D), np.float32); y_s = np.zeros((128, TS, D), np.float32)
    pre = np.zeros((1, 8, G, P), np.float32); pim = np.zeros((1, 8, G, P), np.float32)
    pbuf = np.zeros((1, 8, CB, 512), np.float32)
    sre = np.zeros((1, 128, G, P), np.float32); sim = np.zeros((1, 128, G, P), np.float32)
    sbuf = np.zeros((1, 128, CB, 512), np.float32)
    for c in range(NCORES):
        r = results[c]
        bs = slice(c * NS, (c + 1) * NS)
        y_p[c] = r["y_all"][:SEQ]
        y_s[bs] = r["y_all"][SEQ:].reshape(NS, TS, D)
        stp = r["st_p"]
        pre[0, c] = stp[:64].T; pim[0, c] = stp[64:].T
        sts = r["st_s"]
        sre[0, bs] = sts[:64].transpose(2, 1, 0); sim[0, bs] = sts[64:].transpose(2, 1, 0)
        pbuf[0, c] = r["cc_p"].transpose(2, 1, 0).reshape(CB, 512)
        sbuf[0, bs] = r["cc_s"].transpose(2, 3, 1, 0).reshape(NS, CB, 512)
    return y_p, y_s, pre, pim, pbuf, sre, sim, sbuf


_NC_CACHE = {}


def kernel(**inputs):
    if "nc" not in _NC_CACHE:
        _NC_CACHE["nc"] = build()[0]
    nc = _NC_CACHE["nc"]
    maps = make_in_maps(inputs)
    res = run_bass_kernel_spmd(nc, maps, core_ids=list(range(NCORES)))
    return assemble(res.results, inputs)
```

```python
import math
from contextlib import ExitStack

import numpy as np
import concourse.bass as bass
import concourse.mybir as mybir
from concourse.bass_utils import run_bass_kernel_spmd

F32 = mybir.dt.float32
BF16 = mybir.dt.bfloat16
I32 = mybir.dt.int32
AF = mybir.ActivationFunctionType
ALU = mybir.AluOpType
AX = mybir.AxisListType

NCORES = 8
D = 1024
SEQ = 2048
NS = 16
TS = 4
NTOK = SEQ + NS * TS
NT = 17
G = 32
P = 64
H = 16
CW = 31
CB = 30
NE = 32
DE = 512
EPS = 1e-6
TWO_PI = 2.0 * math.pi
CW1 = 6.28125
CW2 = TWO_PI - 6.28125
BLKS = [(0, 512), (512, 512), (1024, 512), (1536, 512), (2048, 64)]
CAP = 512
NJ = CAP // 128


def tile_rows(i):
    return 128 if i < 16 else 64


class Buf:
    __slots__ = ("name", "w", "r")

    def __init__(self, name):
        self.name = name
        self.w = None
        self.r = []


class S:
    def __init__(self, nc):
        self.nc = nc
        self.eng = {"pe": nc.tensor, "act": nc.scalar, "dve": nc.vector,
                    "pool": nc.gpsimd, "sp": nc.sync}
        self.sems = {}
        self.cnt = {}
        for k in self.eng:
            self.sems[k] = nc.alloc_semaphore("prog_" + k)
            self.cnt[k] = 0
        self.waited = {k: {} for k in self.eng}
        self.bufs = {}

    def _B(self, x):
        if isinstance(x, Buf):
            return x
        b = self.bufs.get(x)
        if b is None:
            b = Buf(x)
            self.bufs[x] = b
        return b

    def _sem(self, name):
        key = "dma_" + name
        if key not in self.sems:
            self.sems[key] = self.nc.alloc_semaphore(key)
            self.cnt[key] = 0
        return key

    def _wait(self, e, key, val):
        if val <= 0:
            return
        w = self.waited[e]
        if w.get(key, 0) >= val:
            return
        w[key] = val
        self.eng[e].wait_ge(self.sems[key], val)

    def _deps(self, e, reads, writes, own=None):
        need = {}

        def want(k, v):
            if need.get(k, 0) < v:
                need[k] = v

        for b in reads:
            b = self._B(b)
            if b.w is not None:
                want(b.w[0], b.w[1])
        same_ok = e in ("act", "dve", "pool")
        for b in writes:
            b = self._B(b)
            if b.w is not None and b.w[0] != own and (b.w[0] != e or same_ok):
                want(b.w[0], b.w[1])
            for (k, v) in b.r:
                if k != e or same_ok:
                    want(k, v)
        for k, v in need.items():
            self._wait(e, k, v)

    def _rec(self, key, tgt, reads, writes):
        for b in reads:
            self._B(b).r.append((key, tgt))
        for b in writes:
            b = self._B(b)
            b.w = (key, tgt)
            b.r = []

    def op(self, e, fn, reads=(), writes=(), signal=True):
        self._deps(e, reads, writes)
        ins = fn(self.eng[e])
        tgt = self.cnt[e] + 1
        if signal:
            ins.then_inc(self.sems[e], 1)
            self.cnt[e] = tgt
        self._rec(e, tgt, reads, writes)
        return ins

    def dma(self, q, out, in_, sem, reads=(), writes=(), **kw):
        key = self._sem(sem)
        self._deps(q, reads, writes, own=key)
        ins = self.eng[q].dma_start(out=out, in_=in_, **kw)
        ins.then_inc(self.sems[key], 16)
        self.cnt[key] += 16
        self._rec(key, self.cnt[key], reads, writes)
        return ins

    def dma_custom(self, q, fn, sem, reads=(), writes=()):
        key = self._sem(sem)
        self._deps(q, reads, writes, own=key)
        ins = fn(self.eng[q])
        ins.then_inc(self.sems[key], 16)
        self.cnt[key] += 16
        self._rec(key, self.cnt[key], reads, writes)
        return ins

    def commit_group(self, names, sem):
        key = "dma_" + sem
        for n in names:
            self._B(n).w = (key, self.cnt[key])

    def barrier(self):
        for e in self.eng:
            for k in self.sems:
                if k != e:
                    self._wait(e, k, self.cnt[k])

    def finish(self, e="sp"):
        for k in self.sems:
            if k != e:
                self._wait(e, k, self.cnt[k])


IN_SPECS = [
    ("x_all", [NTOK, D]), ("c_all", [17, D]),
    ("h0s", [128, G, NS]),
    ("cachef", [128, 4, NS, CB]),
    ("w_ada", [D, 6 * D]), ("b_ada", [1, 6 * D]),
    ("g_mix", [1, D]), ("g_ffn", [1, D]), ("g_fin", [1, D]),
    ("w_in", [D, 1536]), ("w_out", [D, D]), ("w_glu", [512, 512]),
    ("are2", [128, G]), ("aim2", [128, G]), ("ldt2", [128, G]),
    ("bx1", [128, G, H]), ("bx2", [128, G, H]),
    ("ct1", [128, G, H]), ("ct2", [128, G, H]),
    ("vec4", [128, 7, 4]),
    ("wdwT", [128, 4, CW]),
    ("w_rt", [D, 36]), ("b_rt", [1, 36]),
    ("w1", [NE, D, DE]), ("w3", [NE, D, DE]), ("w2", [NE, DE, D]),
    ("c_ident", [128, 128]), ("c_swp", [128, 128]), ("c_rowmask", [128, 8]),
    ("c_sgn", [128, 1]), ("c_jidx", [128, 576]), ("c_m01", [128, 64]),
    ("c_ltri", [128, 128]), ("c_eoff", [128, 32]),
]
OUT_SPECS = [
    ("y_all", [NTOK, D]),
    ("st_p", [128, G]), ("st_s", [128, G, NS]),
    ("cc_p", [128, 4, CB]), ("cc_s", [128, 4, NS, CB]),
]


def build(dbg=False, n_exp=NE):
    nc = bass.Bass("TRN2", target_bir_lowering=False)
    I = {}
    for name, shp in IN_SPECS:
        I[name] = nc.dram_tensor(name, shp, F32, kind="ExternalInput").ap()
    O = {}
    for name, shp in OUT_SPECS:
        O[name] = nc.dram_tensor(name, shp, F32, kind="ExternalOutput").ap()
    mod_scr = nc.dram_tensor("mod_scr", [17, 6 * D], F32, kind="Internal").ap()
    x1_scr = nc.dram_tensor("x1_scr", [NTOK, D], F32, kind="Internal").ap()
    n2_scr = nc.dram_tensor("n2_scr", [NTOK, D], BF16, kind="Internal").ap()
    ys_f_scr = nc.dram_tensor("ys_f_scr", [128, 4, NTOK], F32, kind="Internal").ap()
    xs_scr = nc.dram_tensor("xs_scr", [NE * CAP, D], BF16, kind="Internal").ap()
    ys_scr = nc.dram_tensor("ys_scr", [NE * CAP, D], F32, kind="Internal").ap()
    DBG = {}

    def dbg_out(name, shape):
        if not dbg:
            return None
        DBG[name] = nc.dram_tensor("dbg_" + name, shape, F32, kind="ExternalOutput").ap()
        return DBG[name]

    s = S(nc)
    glob = ExitStack()

    import contextlib

    @contextlib.contextmanager
    def phase():
        with ExitStack() as es_:
            yield es_
            s.barrier()

    def sbt(es, name, shape, dt=F32):
        return es.enter_context(nc.sbuf_tensor("s_" + name, shape, dt))

    def pst(es, name, shape, dt=F32):
        return es.enter_context(nc.psum_tensor("p_" + name, shape, dt))

    with glob:
        ident = sbt(glob, "ident", [128, 128])
        identb = sbt(glob, "identb", [128, 128], BF16)
        sgn = sbt(glob, "sgn", [128, 1])
        epsc = sbt(glob, "epsc", [128, 1])
        halfpi = sbt(glob, "halfpi", [128, 1])
        ones32 = sbt(glob, "ones32", [128, 128])
        vec4 = sbt(glob, "vec4", [128, 7, 4])
        logit = sbt(glob, "logit", [128, NT, 36])
        mix_es = ExitStack()
        glob.enter_context(mix_es)
        mergedT = sbt(mix_es, "mergedT", [128, 8, NTOK], BF16)
        uT = sbt(mix_es, "uT", [128, 4, NTOK], BF16)
        s.dma("sp", ident[:], I["c_ident"], "c0", writes=["ident"])
        s.dma("sp", sgn[:], I["c_sgn"], "c1", writes=["sgn"])
        s.dma("sp", vec4[:], I["vec4"], "c2", writes=["vec4"])
        s.op("dve", lambda e: e.tensor_copy(identb[:], ident[:]), reads=["ident"], writes=["identb"])
        s.op("dve", lambda e: e.memset(epsc[:], EPS), writes=["epsc"])
        s.op("dve", lambda e: e.memset(halfpi[:], math.pi / 2), writes=["halfpi"])
        s.op("dve", lambda e: e.memset(ones32[:], 1.0), writes=["ones32"])
        V_D, V_BGLU, V_BDW, V_LNG, V_LNB, V_GOS, V_GOC = range(7)


        with phase() as es:
            cT = sbt(es, "cT", [128, 8, 17])
            crow = sbt(es, "crow", [17, D])
            bada = sbt(es, "bada", [17, 6 * D])
            modsb = sbt(es, "modsb", [17, 6 * D])
            wa = [sbt(es, "wa%d" % i, [128, 8, 512], BF16) for i in range(3)]
            cTb = sbt(es, "cTb", [128, 8, 17], BF16)
            mp = [pst(es, "mp%d" % i, [17, 512]) for i in range(2)]
            tp = pst(es, "tp0", [128, 8, 17])
            s.dma("sp", crow[:], I["c_all"], "p0a", writes=["crow"])
            s.dma("sp", bada[:], I["b_ada"].to_broadcast([17, 6 * D]), "p0b", writes=["bada"])
            s.op("act", lambda e: e.activation(crow[:], crow[:], AF.Silu), reads=["crow"], writes=["crow"])
            for k in range(8):
                s.op("pe", lambda e: e.transpose(tp[:, k, :], crow[:, k * 128:(k + 1) * 128], ident[0:17, 0:17]),
                     reads=["crow", "ident"], writes=["tp0"])
            s.op("dve", lambda e: e.tensor_copy(cTb[:], tp[:]), reads=["tp0"], writes=["cT"])
            for j in range(12):
                wb_ = wa[j % 3]
                wn = "wa%d" % (j % 3)
                s.dma("pool", wb_[:], I["w_ada"][:, j * 512:(j + 1) * 512].rearrange("(k p) n -> p k n", p=128),
                      wn, writes=[wn])
                mpp = mp[j % 2]
                mn = "mp%d" % (j % 2)
                for k in range(8):
                    s.op("pe", lambda e: e.matmul(mpp[:], cTb[:, k, :], wb_[:, k, :], start=(k == 0), stop=(k == 7)),
                         reads=["cT", wn], writes=[mn], signal=(k == 7))
                s.op("dve", lambda e: e.tensor_tensor(modsb[:, j * 512:(j + 1) * 512], mpp[:], bada[:, j * 512:(j + 1) * 512], ALU.add),
                     reads=[mn, "bada"], writes=["modsb"])
            s.dma("sp", mod_scr, modsb[:], "p0c", reads=["modsb"], writes=["mod_scr"])

        def load_mod(es, which, names, gname=None):
            res = {}
            for idx, nm in zip(which, names):
                tp_ = sbt(es, "modP_" + nm, [128, D])
                ts_ = sbt(es, "modS_" + nm, [64, D])
                s.dma("sp", tp_[:], mod_scr[0:1, idx * D:(idx + 1) * D].to_broadcast([128, D]), "mdp" + nm,
                      reads=["mod_scr"], writes=["modP_" + nm])
                for b in range(NS):
                    s.dma("sp", ts_[b * TS:(b + 1) * TS, :], mod_scr[1 + b:2 + b, idx * D:(idx + 1) * D].to_broadcast([TS, D]), "mds" + nm,
                          reads=["mod_scr"], writes=["modS_" + nm])
                res[nm] = (tp_, ts_)
            return res

        gluB_es = ExitStack()
        glob.enter_context(gluB_es)
        gluB = sbt(gluB_es, "gluB", [128, 4, CB + SEQ], BF16)
        gluS = sbt(gluB_es, "gluS", [128, 4, NS, CB + TS], BF16)
        gluT = sbt(gluB_es, "gluT", [128, 4, CB], F32)
        gluST = sbt(gluB_es, "gluST", [128, 4, NS, CB], F32)
        with phase() as es:
            md = load_mod(es, [0, 1], ["sh1", "sc1"])
            gmix = sbt(es, "gmix", [128, D])
            s.dma("sp", gmix[:], I["g_mix"].to_broadcast([128, D]), "gm", writes=["gmix"])
            for (tl, nm, R) in ((md["sc1"][0], "modP_sc1", 128), (md["sc1"][1], "modS_sc1", 64)):
                s.op("dve", lambda e: e.scalar_tensor_tensor(tl[0:R, :], tl[0:R, :], 1.0, gmix[0:R, :], ALU.add, ALU.mult),
                     reads=[nm, "gmix"], writes=[nm])
            w_in_b = sbt(es, "w_in_b", [128, 8, 1536], BF16)
            s.dma("pool", w_in_b[:], I["w_in"].rearrange("(k p) n -> p k n", p=128), "win", writes=["w_in_b"])
            nTs = [sbt(es, "nT%d" % i, [128, 8, 512], BF16) for i in range(2)]
            xt = [sbt(es, "xt%d" % i, [128, D]) for i in range(3)]
            nt_ = [sbt(es, "nt%d" % i, [128, D]) for i in range(3)]
            ssq = sbt(es, "ssq", [128, NT])
            rstd = sbt(es, "rstd", [128, NT])
            cst = sbt(es, "cst", [128, 4, NS, CB])
            sig = [sbt(es, "sigA%d" % i, [128, 512]) for i in range(2)]
            gl = [sbt(es, "glA%d" % i, [128, 512]) for i in range(2)]
            tpp = [pst(es, "tpA%d" % i, [128, 8, 128]) for i in range(2)]
            pp = [pst(es, "ppA%d" % i, [128, 512]) for i in range(4)]
            s.op("pool", lambda e: e.memset(gluB[:, :, 0:CB], 0.0), writes=["gluB"])
            s.dma("sp", cst[:], I["cachef"], "cst", writes=["cst"])
            s.op("pool", lambda e: e.tensor_copy(gluS[:, :, :, 0:CB], cst[:]), reads=["cst"], writes=["gluS"])
            s.op("pool", lambda e: e.tensor_copy(gluST[:, :, :, 0:CB - TS], cst[:, :, :, TS:CB]), reads=["cst"], writes=["gluST"])
            cnt = 0

            def tiles_stage(bi):
                b0, bw = BLKS[bi]
                nT = nTs[bi % 2]
                nTn = "nT%d" % (bi % 2)
                tiles = range(bi * 4, bi * 4 + 4) if bi < 4 else [16]
                for i in tiles:
                    R = tile_rows(i)
                    c0 = i * 128
                    l0 = c0 - b0
                    xb, xn = xt[i % 3], "xt%d" % (i % 3)
                    nb, nn = nt_[i % 3], "nt%d" % (i % 3)
                    G1 = md["sc1"][0 if i < 16 else 1]
                    S1 = md["sh1"][0 if i < 16 else 1]
                    g1n = "modP_sc1" if i < 16 else "modS_sc1"
                    s1n = "modP_sh1" if i < 16 else "modS_sh1"
                    s.dma("sp", xb[0:R, :], I["x_all"][c0:c0 + R, :], xn, writes=[xn])
                    s.op("act", lambda e: e.activation(nb[0:R, :], xb[0:R, :], AF.Square, accum_out=ssq[0:R, i:i + 1]),
                         reads=[xn], writes=[nn, "ssq"])
                    s.op("act", lambda e: e.activation(rstd[0:R, i:i + 1], ssq[0:R, i:i + 1], AF.Sqrt, scale=1.0 / D, bias=epsc[0:R, :]),
                         reads=["ssq", "epsc"], writes=["rstd"])
                    s.op("dve", lambda e: e.reciprocal(rstd[0:R, i:i + 1], rstd[0:R, i:i + 1]), reads=["rstd"], writes=["rstd"])
                    s.op("dve", lambda e: e.scalar_tensor_tensor(nb[0:R, :], xb[0:R, :], rstd[0:R, i:i + 1], G1[0:R, :], ALU.mult, ALU.mult),
                         reads=[xn, "rstd", g1n], writes=[nn])
                    s.op("dve", lambda e: e.tensor_tensor(nb[0:R, :], nb[0:R, :], S1[0:R, :], ALU.add), reads=[nn, s1n], writes=[nn])
                    tpb, tpn = tpp[i % 2], "tpA%d" % (i % 2)
                    for k in range(8):
                        s.op("pe", lambda e: e.transpose(tpb[:, k, 0:R], nb[0:R, k * 128:(k + 1) * 128], ident[0:R, 0:R]),
                             reads=[nn, "ident"], writes=[tpn], signal=(k == 7))
                    s.op("act", lambda e: e.copy(nT[:, :, l0:l0 + R], tpb[:, :, 0:R]), reads=[tpn], writes=[nTn])

            def inproj_stage(bi):
                nonlocal cnt
                b0, bw = BLKS[bi]
                nT = nTs[bi % 2]
                nTn = "nT%d" % (bi % 2)
                for m in range(4):
                    pb, pn = pp[cnt % 4], "ppA%d" % (cnt % 4)
                    cnt += 1
                    for k in range(8):
                        s.op("pe", lambda e: e.matmul(pb[:, 0:bw], w_in_b[:, k, m * 128:(m + 1) * 128], nT[:, k, 0:bw],
                                                      start=(k == 0), stop=(k == 7)),
                             reads=["w_in_b", nTn], writes=[pn], signal=(k == 7))
                    s.op("act", lambda e: e.copy(uT[:, m, b0:b0 + bw], pb[:, 0:bw]), reads=[pn], writes=["uT"])
                for m in range(4):
                    pa, pan = pp[cnt % 4], "ppA%d" % (cnt % 4)
                    cnt += 1
                    pg, pgn = pp[cnt % 4], "ppA%d" % (cnt % 4)
                    cnt += 1
                    for k in range(8):
                        s.op("pe", lambda e: e.matmul(pa[:, 0:bw], w_in_b[:, k, 512 + m * 128:512 + (m + 1) * 128], nT[:, k, 0:bw],
                                                      start=(k == 0), stop=(k == 7)),
                             reads=["w_in_b", nTn], writes=[pan], signal=(k == 7))
                    for k in range(8):
                        s.op("pe", lambda e: e.matmul(pg[:, 0:bw], w_in_b[:, k, 1024 + m * 128:1024 + (m + 1) * 128], nT[:, k, 0:bw],
                                                      start=(k == 0), stop=(k == 7)),
                             reads=["w_in_b", nTn], writes=[pgn], signal=(k == 7))
                    sg, sgn_ = sig[m % 2], "sigA%d" % (m % 2)
                    gg, ggn = gl[m % 2], "glA%d" % (m % 2)
                    s.op("act", lambda e: e.activation(sg[:, 0:bw], pg[:, 0:bw], AF.Sigmoid), reads=[pgn], writes=[sgn_])
                    s.op("dve", lambda e: e.tensor_tensor(gg[:, 0:bw], pa[:, 0:bw], sg[:, 0:bw], ALU.mult), reads=[pan, sgn_], writes=[ggn])
                    if b0 < SEQ:
                        s.op("act", lambda e: e.copy(gluB[:, m, CB + b0:CB + b0 + bw], gg[:, 0:bw]), reads=[ggn], writes=["gluB"])
                        if b0 + bw == SEQ:
                            s.op("pool", lambda e: e.tensor_copy(gluT[:, m, :], gg[:, bw - CB:bw]), reads=[ggn], writes=["gluT"])
                    else:
                        s.op("pool", lambda e: e.tensor_copy(gluS[:, m, :, CB:CB + TS], gg[:, 0:bw].rearrange("p (b t) -> p b t", t=TS)),
                             reads=[ggn], writes=["gluS"])
                        s.op("pool", lambda e: e.tensor_copy(gluST[:, m, :, CB - TS:CB], gg[:, 0:bw].rearrange("p (b t) -> p b t", t=TS)),
                             reads=[ggn], writes=["gluST"])

            for bi in range(len(BLKS) + 1):
                if bi < len(BLKS):
                    tiles_stage(bi)
                if bi >= 1:
                    inproj_stage(bi - 1)
            s.dma("sp", O["cc_p"], gluT[:], "occp", reads=["gluT"], writes=["o_cc_p"])
            s.dma("sp", O["cc_s"], gluST[:], "occs", reads=["gluST"], writes=["o_cc_s"])
            if dbg:
                d_uT = dbg_out("uT", [128, 4, NTOK])
                dtmp = sbt(es, "dtmp", [128, 4, NTOK])
                s.op("dve", lambda e: e.tensor_copy(dtmp[:], uT[:]), reads=["uT"], writes=["dtmp"])
                s.dma("sp", d_uT, dtmp[:], "dbg0", reads=["dtmp"], writes=["d_uT"])

        with phase() as es:
            wdw = sbt(es, "wdw", [128, 4, CW])
            s.dma("sp", wdw[:], I["wdwT"], "wdw", writes=["wdw"])
            zt = sbt(es, "zt", [128, 2048], BF16)
            s.op("pool", lambda e: e.memset(zt[:], 0.0), writes=["zt"])
            xs_v = xs_scr.rearrange("(p q) d -> p (q d)", p=128)
            for q_ in range(NE * CAP // 128 // 2):
                s.dma("sp", xs_v[:, q_ * 2048:(q_ + 1) * 2048], zt[:], "zf", reads=["zt"], writes=["xs_zf"])

            diag = sbt(es, "diag", [128, 4, CW, 128], BF16)
            for m in range(4):
                for k in range(CW):
                    if (m * CW + k) % 2 == 0:
                        s.op("dve", lambda e: e.tensor_scalar(diag[:, m, k, :], ident[:], wdw[:, m, k:k + 1], None, ALU.mult),
                             reads=["ident", "wdw"], writes=["diag%d_%d" % (m, k)])
                    else:
                        s.op("act", lambda e: e.activation(diag[:, m, k, :], ident[:], AF.Copy, scale=wdw[:, m, k:k + 1]),
                             reads=["ident", "wdw"], writes=["diag%d_%d" % (m, k)])
            cps = [pst(es, "cps%d" % i, [128, 512]) for i in range(4)]
            stp = [pst(es, "stp%d" % i, [128, 512]) for i in range(3)]
            xc = [sbt(es, "xc%d" % i, [128, 512]) for i in range(4)]
            xq = [sbt(es, "xq%d" % i, [128, 512]) for i in range(4)]
            mu = sbt(es, "mu", [128, 512])
            var = sbt(es, "var", [128, 512])
            rs = sbt(es, "rsB", [128, 512])
            rs2 = sbt(es, "rs2B", [128, 512])
            for (b0, bw) in BLKS:
                for m in range(4):
                    for k in range(CW):
                        if b0 < SEQ:
                            rhs = gluB[:, m, b0 + k:b0 + k + bw]
                            rd = "gluB"
                        else:
                            rhs = gluS[:, m, :, k:k + TS]
                            rd = "gluS"
                        outp = cps[m][:, 0:bw] if b0 < SEQ else cps[m][:, 0:bw].rearrange("p (b t) -> p b t", t=TS)
                        s.op("pe", lambda e: e.matmul(outp, diag[:, m, k, :], rhs, start=(k == 0), stop=(k == CW - 1)),
                             reads=["diag%d_%d" % (m, k), rd], writes=["cps%d" % m], signal=(k == CW - 1))
                    s.op("act", lambda e: e.activation(xc[m][:, 0:bw], cps[m][:, 0:bw], AF.Identity, bias=vec4[:, V_BDW, m:m + 1]),
                         reads=["cps%d" % m, "vec4"], writes=["xc%d" % m])
                    s.op("pool", lambda e: e.tensor_tensor(xq[m][:, 0:bw], xc[m][:, 0:bw], xc[m][:, 0:bw], ALU.mult),
                         reads=["xc%d" % m], writes=["xq%d" % m])
                for m in range(4):
                    s.op("pe", lambda e: e.matmul(stp[0][:, 0:bw], ones32[:], xc[m][:, 0:bw], start=(m == 0), stop=(m == 3)),
                         reads=["ones32", "xc%d" % m], writes=["stp0"], signal=(m == 3))
                for m in range(4):
                    s.op("pe", lambda e: e.matmul(stp[1][:, 0:bw], ones32[:], xq[m][:, 0:bw], start=(m == 0), stop=(m == 3)),
                         reads=["ones32", "xq%d" % m], writes=["stp1"], signal=(m == 3))
                s.op("act", lambda e: e.mul(mu[:, 0:bw], stp[0][:, 0:bw], 1.0 / 512), reads=["stp0"], writes=["mu"])
                s.op("dve", lambda e: e.tensor_tensor(var[:, 0:bw], mu[:, 0:bw], mu[:, 0:bw], ALU.mult), reads=["mu"], writes=["var"])
                s.op("dve", lambda e: e.scalar_tensor_tensor(var[:, 0:bw], stp[1][:, 0:bw], 1.0 / 512, var[:, 0:bw], ALU.mult, ALU.subtract),
                     reads=["stp1", "var"], writes=["var"])
                s.op("act", lambda e: e.activation(rs[:, 0:bw], var[:, 0:bw], AF.Sqrt, bias=epsc[:, :]), reads=["var", "epsc"], writes=["rsB"])
                s.op("dve", lambda e: e.reciprocal(rs[:, 0:bw], rs[:, 0:bw]), reads=["rsB"], writes=["rsB"])
                for m in range(4):
                    s.op("dve", lambda e: e.tensor_tensor(xc[m][:, 0:bw], xc[m][:, 0:bw], mu[:, 0:bw], ALU.subtract),
                         reads=["xc%d" % m, "mu"], writes=["xc%d" % m])
                    s.op("dve", lambda e: e.tensor_tensor(xc[m][:, 0:bw], xc[m][:, 0:bw], rs[:, 0:bw], ALU.mult),
                         reads=["xc%d" % m, "rsB"], writes=["xc%d" % m])
                    s.op("act", lambda e: e.activation(xc[m][:, 0:bw], xc[m][:, 0:bw], AF.Silu, scale=vec4[:, V_LNG, m:m + 1], bias=vec4[:, V_LNB, m:m + 1]),
                         reads=["xc%d" % m, "vec4"], writes=["xc%d" % m])
                    s.op("pool", lambda e: e.tensor_tensor(xq[m][:, 0:bw], xc[m][:, 0:bw], xc[m][:, 0:bw], ALU.mult),
                         reads=["xc%d" % m], writes=["xq%d" % m])
                for m in range(4):
                    s.op("pe", lambda e: e.matmul(stp[2][:, 0:bw], ones32[:], xq[m][:, 0:bw], start=(m == 0), stop=(m == 3)),
                         reads=["ones32", "xq%d" % m], writes=["stp2"], signal=(m == 3))
                s.op("act", lambda e: e.activation(rs2[:, 0:bw], stp[2][:, 0:bw], AF.Sqrt, scale=1.0 / 512, bias=epsc[:, :]),
                     reads=["stp2", "epsc"], writes=["rs2B"])
                s.op("dve", lambda e: e.reciprocal(rs2[:, 0:bw], rs2[:, 0:bw]), reads=["rs2B"], writes=["rs2B"])
                for m in range(4):
                    s.op("dve", lambda e: e.scalar_tensor_tensor(mergedT[:, 4 + m, b0:b0 + bw], xc[m][:, 0:bw], vec4[:, V_GOC, m:m + 1], rs2[:, 0:bw], ALU.mult, ALU.mult),
                         reads=["xc%d" % m, "vec4", "rs2B"], writes=["mergedT"])
        s.barrier()
        gluB_es.close()

        with phase() as es:
            BBp = sbt(es, "BBp", [128, G, 128], BF16); BBq = sbt(es, "BBq", [128, G, 128], BF16)
            CAp = sbt(es, "CAp", [128, G, 128], BF16); CBp = sbt(es, "CBp", [128, G, 128], BF16)
            dgD = sbt(es, "dgD", [128, 4, 128], BF16)
            swp = sbt(es, "swp", [128, 128]); jidx = sbt(es, "jidx", [128, 576]); m01 = sbt(es, "m01", [128, 64])
            h0s = sbt(es, "h0s", [128, G, NS])
            mag = sbt(es, "mag", [128, G]); th = sbt(es, "th", [128, G])
            hcar = sbt(es, "hcar", [128, 5, G])
            stS = sbt(es, "stS", [128, G, NS])
            ysSt = [sbt(es, "ysSt%d" % i, [128, 512]) for i in range(2)]
            ysB = sbt(es, "ysB", [128, 4, NTOK], BF16)
            wgl = sbt(es, "wgl", [128, 4, 512], BF16)
            pre_es = ExitStack()
            are = sbt(pre_es, "are", [128, G]); aim = sbt(pre_es, "aim", [128, G]); ldt = sbt(pre_es, "ldt", [128, G])
            s.dma("sp", are[:], I["are2"], "spx", writes=["are"])
            s.dma("sp", aim[:], I["aim2"], "spx", writes=["aim"])
            s.dma("sp", ldt[:], I["ldt2"], "spx", writes=["ldt"])
            bx1 = sbt(pre_es, "bx1", [128, G, H]); bx2 = sbt(pre_es, "bx2", [128, G, H])
            ct1 = sbt(pre_es, "ct1", [128, G, H]); ct2 = sbt(pre_es, "ct2", [128, G, H])
            s.dma("sp", bx1[:], I["bx1"], "spx", writes=["bx1"])
            s.dma("sp", bx2[:], I["bx2"], "spx", writes=["bx2"])
            s.dma("sp", ct1[:], I["ct1"], "spx", writes=["ct1"])
            s.dma("sp", ct2[:], I["ct2"], "spx", writes=["ct2"])
            rowmask = sbt(pre_es, "rowmask", [128, 8])
            pass
            pass
            s.dma("sp", swp[:], I["c_swp"], "spx", writes=["swp"])
            s.dma("sp", rowmask[:], I["c_rowmask"], "spx", writes=["rowmask"])
            s.dma("sp", jidx[:], I["c_jidx"], "spx", writes=["jidx"])
            s.dma("sp", m01[:], I["c_m01"], "spx", writes=["m01"])
            s.dma("sp", h0s[:], I["h0s"], "spx", writes=["h0s"])
            s.commit_group(["are", "aim", "ldt", "bx1", "bx2", "ct1", "ct2", "swp", "rowmask", "jidx", "m01", "h0s"], "spx")
            PR = "ssmpar"
            dt_ = sbt(pre_es, "dt_", [128, G])
            t1 = sbt(pre_es, "t1", [128, G]); t2 = sbt(pre_es, "t2", [128, G]); ki = sbt(pre_es, "ki", [128, G], I32)
            cs = sbt(pre_es, "cs", [128, G]); sn = sbt(pre_es, "sn", [128, G])
            abr = sbt(pre_es, "abr", [128, G]); abi = sbt(pre_es, "abi", [128, G]); den = sbt(pre_es, "den", [128, G])
            cfr = sbt(pre_es, "cfr", [128, G]); cfi = sbt(pre_es, "cfi", [128, G]); cfin = sbt(pre_es, "cfin", [128, G])

            def dv(fn, rd=(), wr=()):
                s.op("dve", fn, reads=[PR] + list(rd), writes=[PR] + list(wr))

            def ac(fn, rd=(), wr=()):
                s.op("act", fn, reads=[PR] + list(rd), writes=[PR] + list(wr))

            dv(lambda e: e.tensor_scalar(are[:], are[:], -1e-4, None, ALU.min), rd=["are"])
            ac(lambda e: e.activation(dt_[:], ldt[:], AF.Exp), rd=["ldt"])
            dv(lambda e: e.tensor_tensor(t1[:], are[:], dt_[:], ALU.mult))
            ac(lambda e: e.activation(mag[:], t1[:], AF.Exp))
            dv(lambda e: e.tensor_tensor(th[:], aim[:], dt_[:], ALU.mult), rd=["aim"])
            dv(lambda e: e.tensor_scalar(t1[:], th[:], 1.0 / TWO_PI, None, ALU.mult))
            dv(lambda e: e.tensor_copy(ki[:], t1[:]))
            dv(lambda e: e.tensor_copy(t1[:], ki[:]))
            dv(lambda e: e.scalar_tensor_tensor(t2[:], t1[:], -CW1, th[:], ALU.mult, ALU.add))
            dv(lambda e: e.scalar_tensor_tensor(t2[:], t1[:], -CW2, t2[:], ALU.mult, ALU.add))
            dv(lambda e: e.tensor_scalar(t2[:], t2[:], math.pi, -math.pi, ALU.min, ALU.max))
            ac(lambda e: e.activation(sn[:], t2[:], AF.Sin))
            ac(lambda e: e.activation(t2[:], t2[:], AF.Abs))
            ac(lambda e: e.activation(cs[:], t2[:], AF.Sin, scale=-1.0, bias=halfpi[:, :]), rd=["halfpi"])
            dv(lambda e: e.tensor_tensor(abr[:], mag[:], cs[:], ALU.mult))
            dv(lambda e: e.tensor_tensor(abi[:], mag[:], sn[:], ALU.mult))
            dv(lambda e: e.tensor_tensor(den[:], are[:], are[:], ALU.mult))
            dv(lambda e: e.tensor_tensor(t1[:], aim[:], aim[:], ALU.mult))
            dv(lambda e: e.tensor_tensor(den[:], den[:], t1[:], ALU.add))
            dv(lambda e: e.reciprocal(den[:], den[:]))
            dv(lambda e: e.tensor_scalar(t1[:], abr[:], -1.0, None, ALU.add))
            dv(lambda e: e.tensor_tensor(cfr[:], t1[:], are[:], ALU.mult))
            dv(lambda e: e.tensor_tensor(t2[:], abi[:], aim[:], ALU.mult))
            dv(lambda e: e.tensor_tensor(cfr[:], cfr[:], t2[:], ALU.add))
            dv(lambda e: e.tensor_tensor(cfr[:], cfr[:], den[:], ALU.mult))
            dv(lambda e: e.tensor_tensor(cfi[:], abi[:], are[:], ALU.mult))
            dv(lambda e: e.tensor_tensor(t2[:], t1[:], aim[:], ALU.mult))
            dv(lambda e: e.tensor_tensor(cfi[:], cfi[:], t2[:], ALU.subtract))
            dv(lambda e: e.tensor_tensor(cfi[:], cfi[:], den[:], ALU.mult))
            dv(lambda e: e.tensor_scalar(cfin[:], cfi[:], sgn[:, 0:1], -1.0, ALU.mult, ALU.mult), rd=["sgn"])
            bbs = sbt(pre_es, "bbs", [128, G, H]); bbw = sbt(pre_es, "bbw", [128, G, H]); tb = sbt(pre_es, "tb", [128, G, H])
            cfr_b = cfr[:].unsqueeze(2).to_broadcast([128, G, H])
            cfin_b = cfin[:].unsqueeze(2).to_broadcast([128, G, H])
            dv(lambda e: e.tensor_tensor(bbs[:], bx1[:], cfr_b, ALU.mult), rd=["bx1"])
            dv(lambda e: e.tensor_tensor(tb[:], bx2[:], cfin_b, ALU.mult), rd=["bx2"])
            dv(lambda e: e.tensor_tensor(bbs[:], bbs[:], tb[:], ALU.add))
            dv(lambda e: e.tensor_tensor(bbw[:], bx2[:], cfr_b, ALU.mult))
            dv(lambda e: e.tensor_tensor(tb[:], bx1[:], cfin_b, ALU.mult))
            dv(lambda e: e.tensor_tensor(bbw[:], bbw[:], tb[:], ALU.subtract))
            pass
            tpc = pst(pre_es, "tpc", [128, 2, 128])
            for gc in range(4):
                s.op("pe", lambda e: e.transpose(tpc[:, 0, :], bbs[:, gc * 8:(gc + 1) * 8, :].rearrange("p g i -> p (g i)"), ident[:]),
                     reads=[PR, "ident"], writes=["tpc"])
                s.op("pe", lambda e: e.transpose(tpc[:, 1, :], bbw[:, gc * 8:(gc + 1) * 8, :].rearrange("p g i -> p (g i)"), ident[:]),
                     reads=[PR, "ident"], writes=["tpc"])
                for g8 in range(8):
                    g = gc * 8 + g8
                    s.op("dve", lambda e: e.tensor_scalar(BBp[:, g, :], tpc[:, 0, :], rowmask[:, g8:g8 + 1], None, ALU.mult),
                         reads=["tpc", "rowmask"], writes=["BBp"])
                    s.op("dve", lambda e: e.tensor_scalar(BBq[:, g, :], tpc[:, 1, :], rowmask[:, g8:g8 + 1], None, ALU.mult),
                         reads=["tpc", "rowmask"], writes=["BBq"])
            pass
            s.op("pool", lambda e: e.memset(CAp[:], 0.0), writes=["CAp"])
            s.op("pool", lambda e: e.memset(CBp[:], 0.0), writes=["CBp"])
            for g8 in range(8):
                s.op("dve", lambda e: e.tensor_scalar(CAp[:, g8::8, g8 * 16:(g8 + 1) * 16], ct1[:, g8::8, :], sgn[:, 0:1], None, ALU.mult),
                     reads=["ct1", "sgn", "CAp"], writes=["CAp"])
                s.op("dve", lambda e: e.tensor_scalar(CBp[:, g8::8, g8 * 16:(g8 + 1) * 16], ct2[:, g8::8, :], -1.0, None, ALU.mult),
                     reads=["ct2", "CBp"], writes=["CBp"])
            pass
            for m in range(4):
                s.op("dve", lambda e: e.tensor_scalar(dgD[:, m, :], ident[:], vec4[:, V_D, m:m + 1], None, ALU.mult),
                     reads=["ident", "vec4"], writes=["dgD"])

            s.barrier()
            pre_es.close()
            NI = 3
            NB = 2 * NI
            Yp = [pst(es, "Yp%d" % i, [128, 512]) for i in range(5)]
            Pp = [pst(es, "Pp%d" % i, [128, 512]) for i in range(2)]
            Cp = pst(es, "Cp", [128, 512])
            ang = [sbt(es, "ang%d" % i, [128, 576]) for i in range(NI)]
            kf = [sbt(es, "kf%d" % i, [128, 576]) for i in range(NB)]
            kint = sbt(es, "kint", [128, 576], I32)
            ctab = [sbt(es, "ctab%d" % i, [128, 576]) for i in range(NB)]
            stab = [sbt(es, "stab%d" % i, [128, 576]) for i in range(NB)]
            rotP = [sbt(es, "rotP%d" % i, [128, 128]) for i in range(NB)]
            rotS = [sbt(es, "rotS%d" % i, [128, 128]) for i in range(NB)]
            magS = [sbt(es, "magS%d" % i, [128, 64]) for i in range(NB)]
            NW_ = NI
            p1s = [sbt(es, "p1s%d" % i, [128, 512]) for i in range(NW_)]
            p2s = [sbt(es, "p2s%d" % i, [128, 512]) for i in range(NW_)]
            bp = [sbt(es, "bp%d" % i, [128, 512]) for i in range(NW_)]
            zz = [sbt(es, "zz%d" % i, [128, 512]) for i in range(NW_)]
            zc = [sbt(es, "zc%d" % i, [128, 512], BF16) for i in range(NW_)]
            zs = [sbt(es, "zs%d" % i, [128, 512], BF16) for i in range(NW_)]
            zc_f = sbt(es, "zcf", [128, 512])

            def build_tables_multi(gs):
                for x, g in enumerate(gs):
                    s.op("act", lambda e: e.activation(ang[x][:], jidx[:], AF.Copy, scale=th[:, g:g + 1]), reads=["jidx", PR], writes=["ang%d" % x])
                for x, g in enumerate(gs):
                    A = "ang%d" % x
                    s.op("dve", lambda e: e.tensor_scalar(kint[:], ang[x][:], 1.0 / TWO_PI, None, ALU.mult), reads=[A], writes=["kint"])
                    s.op("dve", lambda e: e.scalar_tensor_tensor(ang[x][:], kint[:], -CW1, ang[x][:], ALU.mult, ALU.add), reads=["kint", A], writes=[A])
                    s.op("dve", lambda e: e.scalar_tensor_tensor(ang[x][:], kint[:], -CW2, ang[x][:], ALU.mult, ALU.add), reads=["kint", A], writes=[A])
                    s.op("dve", lambda e: e.tensor_scalar(ang[x][:], ang[x][:], math.pi, -math.pi, ALU.min, ALU.max), reads=[A], writes=[A])
                for x, g in enumerate(gs):
                    q = g % NB
                    T = "tab%d" % q
                    A = "ang%d" % x
                    s.op("act", lambda e: e.activation(stab[q][:], ang[x][:], AF.Sin, scale=sgn[:, 0:1]), reads=[A, "sgn"], writes=[T])
                    s.op("act", lambda e: e.activation(kf[q][:], ang[x][:], AF.Sin), reads=[A], writes=["kf%d" % q])
                    s.op("act", lambda e: e.activation(ang[x][:], ang[x][:], AF.Abs), reads=[A, T], writes=[A])
                    s.op("act", lambda e: e.activation(ctab[q][:], ang[x][:], AF.Sin, scale=-1.0, bias=halfpi[:, :]), reads=[A, "halfpi"], writes=[T])

            def build_rots(g):
                q = g % NB
                T = "tab%d" % q
                for (rot, rn, ji) in ((rotP[q], "rotP%d" % q, 511), (rotS[q], "rotS%d" % q, 3)):
                    s.op("dve", lambda e: e.tensor_scalar(rot[:], ident[:], ctab[q][:, ji:ji + 1], None, ALU.mult), reads=["ident", T], writes=[rn])
                    s.op("dve", lambda e: e.scalar_tensor_tensor(rot[:], swp[:], stab[q][:, ji:ji + 1], rot[:], ALU.mult, ALU.add), reads=["swp", T, rn], writes=[rn])
                s.op("dve", lambda e: e.tensor_scalar(magS[q][:], m01[:], mag[:, g:g + 1], None, ALU.mult), reads=["m01", PR], writes=["magS%d" % q])

            def step_front(g, gc, bi, b0, bw, w):
                q = g % NB
                T = "tab%d" % q
                samp = (b0 >= SEQ)
                to = 512 if samp else 0
                s.op("pe", lambda e: e.matmul(Pp[0][:, 0:bw], BBp[:, g, :], uT[:, gc, b0:b0 + bw], start=True, stop=True),
                     reads=["BBp", "uT"], writes=["Pp0"])
                s.op("pe", lambda e: e.matmul(Pp[1][:, 0:bw], BBq[:, g, :], uT[:, gc, b0:b0 + bw], start=True, stop=True),
                     reads=["BBq", "uT"], writes=["Pp1"])
                s.op("act", lambda e: e.copy(p1s[w][:, 0:bw], Pp[0][:, 0:bw]), reads=["Pp0"], writes=["p1s%d" % w])
                s.op("act", lambda e: e.copy(p2s[w][:, 0:bw], Pp[1][:, 0:bw]), reads=["Pp1"], writes=["p2s%d" % w])
                s.op("dve", lambda e: e.tensor_tensor(bp[w][:, 0:bw], p1s[w][:, 0:bw], ctab[q][:, to:to + bw], ALU.mult),
                     reads=["p1s%d" % w, T], writes=["bp%d" % w])
                s.op("dve", lambda e: e.tensor_tensor(p2s[w][:, 0:bw], p2s[w][:, 0:bw], stab[q][:, to:to + bw], ALU.mult),
                     reads=["p2s%d" % w, T], writes=["p2s%d" % w])

            def step_scan(g, gc, bi, b0, bw, w):
                q = g % NB
                samp = (b0 >= SEQ)
                s.op("dve", lambda e: e.tensor_tensor(bp[w][:, 0:bw], bp[w][:, 0:bw], p2s[w][:, 0:bw], ALU.add),
                     reads=["bp%d" % w, "p2s%d" % w], writes=["bp%d" % w])
                if samp:
                    bpv = bp[w][:, 0:bw].rearrange("p (b t) -> p b t", t=TS)[:, :, 0]
                    s.op("dve", lambda e: e.scalar_tensor_tensor(bpv, h0s[:, g, :], mag[:, g:g + 1], bpv, ALU.mult, ALU.add),
                         reads=["h0s", PR, "bp%d" % w], writes=["bp%d" % w])
                    s.op("dve", lambda e: e.tensor_tensor_scan(zz[w][:, 0:bw], magS[q][:, 0:bw], bp[w][:, 0:bw], 0.0, ALU.mult, ALU.add),
                         reads=["magS%d" % q, "bp%d" % w], writes=["zz%d" % w])
                    s.op("pe", lambda e: e.matmul(Cp[:, 128:128 + NS], rotS[q][:], zz[w][:, 0:bw].rearrange("p (b t) -> p b t", t=TS)[:, :, TS - 1], start=True, stop=True),
                         reads=["rotS%d" % q, "zz%d" % w], writes=["Cp"])
                    s.op("act", lambda e: e.copy(stS[:, g, :], Cp[:, 128:128 + NS]), reads=["Cp"], writes=["stS"])
                else:
                    init = 0.0 if bi == 0 else hcar[:, bi - 1, g:g + 1]
                    s.op("dve", lambda e: e.tensor_tensor_scan(zz[w][:, 0:bw], mag[:, g:g + 1].to_broadcast([128, bw]), bp[w][:, 0:bw], init, ALU.mult, ALU.add),
                         reads=[PR, "bp%d" % w, "hcar%d" % g], writes=["zz%d" % w])
                    s.op("pe", lambda e: e.matmul(Cp[:, 160 + bi:161 + bi], rotP[q][:], zz[w][:, bw - 1:bw], start=True, stop=True),
                         reads=["rotP%d" % q, "zz%d" % w], writes=["Cp"])
                    s.op("act", lambda e: e.copy(hcar[:, bi, g:g + 1], Cp[:, 160 + bi:161 + bi]), reads=["Cp"], writes=["hcar%d" % g])

            def step_back(g, gc, bi, b0, bw, w, last):
                q = g % NB
                T = "tab%d" % q
                samp = (b0 >= SEQ)
                to = 512 if samp else 0
                s.op("dve", lambda e: e.tensor_tensor(zc[w][:, 0:bw], zz[w][:, 0:bw], ctab[q][:, to:to + bw], ALU.mult),
                     reads=["zz%d" % w, T], writes=["zc%d" % w])
                s.op("dve", lambda e: e.tensor_tensor(zs[w][:, 0:bw], zz[w][:, 0:bw], kf[q][:, to:to + bw], ALU.mult),
                     reads=["zz%d" % w, "kf%d" % q], writes=["zs%d" % w])
                s.op("pe", lambda e: e.matmul(Yp[bi][:, 0:bw], CAp[:, g, :], zc[w][:, 0:bw], start=False, stop=False),
                     reads=["CAp", "zc%d" % w], writes=["Yp%d" % bi], signal=False)
                s.op("pe", lambda e: e.matmul(Yp[bi][:, 0:bw], CBp[:, g, :], zs[w][:, 0:bw], start=False, stop=last),
                     reads=["CBp", "zs%d" % w], writes=["Yp%d" % bi], signal=True)

            build_tables_multi(list(range(min(NI, G))))
            for gi_ in range(min(NI, G)):
                build_rots(gi_)
            for gc in range(4):
                for bi, (b0, bw) in enumerate(BLKS):
                    s.op("pe", lambda e: e.matmul(Yp[bi][:, 0:bw], dgD[:, gc, :], uT[:, gc, b0:b0 + bw], start=True, stop=False),
                         reads=["dgD", "uT"], writes=["Yp%d" % bi], signal=False)
                for gq in range(0, 8, NI):
                    grp = [gc * 8 + gq + x for x in range(NI) if gq + x < 8]
                    build_tables_multi([g + NI for g in grp if g + NI < G])
                    for bi, (b0, bw) in enumerate(BLKS):
                        ws = [x for x in range(len(grp))]
                        if bi == 1:
                            for g in grp:
                                if g + NI < G:
                                    build_rots(g + NI)
                        for x, g in enumerate(grp):
                            step_front(g, gc, bi, b0, bw, ws[x])
                        for x, g in enumerate(grp):
                            step_scan(g, gc, bi, b0, bw, ws[x])
                        for x, g in enumerate(grp):
                            step_back(g, gc, bi, b0, bw, ws[x], last=(gq + x == 7))
                for bi, (b0, bw) in enumerate(BLKS):
                    yw = (gc * 5 + bi) % 2
                    s.op("act", lambda e: e.activation(ysSt[yw][:, 0:bw], Yp[bi][:, 0:bw], AF.Gelu_apprx_tanh),
                         reads=["Yp%d" % bi], writes=["ysSt%d" % yw])
                    s.op("dve", lambda e: e.tensor_copy(ysB[:, gc, b0:b0 + bw], ysSt[yw][:, 0:bw]), reads=["ysSt%d" % yw], writes=["ysB"])
                    s.dma("sp", ys_f_scr[:, gc, b0:b0 + bw], ysSt[yw][:, 0:bw], "ysfst%d" % yw, reads=["ysSt%d" % yw], writes=["ysf_%d_%d" % (gc, bi)])
            s.dma("sp", O["st_p"], hcar[:, 3, :], "ostp", reads=["hcar%d" % g_ for g_ in range(G)], writes=["o_st_p"])
            s.dma("sp", O["st_s"], stS[:], "osts", reads=["stS"], writes=["o_st_s"])
            s.dma("pool", wgl[:], I["w_glu"].rearrange("(k p) n -> p k n", p=128), "wgl", writes=["wgl"])
            sgg = [p1s[0], p1s[1], p2s[0], p2s[1]]
            sqq = [bp[0], bp[1], bp[2], p1s[2]]
            SGN = ["p1s0", "p1s1", "p2s0", "p2s1"]
            SQN = ["bp0", "bp1", "bp2", "p1s2"]
            rs3 = ang[0]
            ysFb = [zz[0], zz[1], zz[2], zc_f]
            YFN = ["zz0", "zz1", "zz2", "zcf"]
            for bi, (b0, bw) in enumerate(BLKS):
                for m in range(4):
                    s.dma("sp", ysFb[m][:, 0:bw], ys_f_scr[:, m, b0:b0 + bw], "ysfld%d" % m, reads=["ysf_%d_%d" % (m, bi)], writes=[YFN[m]])
                for m in range(4):
                    for k in range(4):
                        s.op("pe", lambda e: e.matmul(Yp[m][:, 0:bw], wgl[:, k, m * 128:(m + 1) * 128], ysB[:, k, b0:b0 + bw], start=(k == 0), stop=(k == 3)),
                             reads=["wgl", "ysB"], writes=["Yp%d" % m], signal=(k == 3))
                    s.op("act", lambda e: e.activation(sgg[m][:, 0:bw], Yp[m][:, 0:bw], AF.Sigmoid, bias=vec4[:, V_BGLU, m:m + 1]),
                         reads=["Yp%d" % m, "vec4"], writes=[SGN[m]])
                    s.op("dve", lambda e: e.tensor_tensor(sgg[m][:, 0:bw], sgg[m][:, 0:bw], ysFb[m][:, 0:bw], ALU.mult),
                         reads=[SGN[m], YFN[m]], writes=[SGN[m]])
                    s.op("pool", lambda e: e.tensor_tensor(sqq[m][:, 0:bw], sgg[m][:, 0:bw], sgg[m][:, 0:bw], ALU.mult),
                         reads=[SGN[m]], writes=[SQN[m]])
                for m in range(4):
                    s.op("pe", lambda e: e.matmul(Pp[0][:, 0:bw], ones32[:], sqq[m][:, 0:bw], start=(m == 0), stop=(m == 3)),
                         reads=["ones32", SQN[m]], writes=["Pp0"], signal=(m == 3))
                s.op("act", lambda e: e.activation(rs3[:, 0:bw], Pp[0][:, 0:bw], AF.Sqrt, scale=1.0 / 512, bias=epsc[:, :]),
                     reads=["Pp0", "epsc"], writes=["ang0"])
                s.op("dve", lambda e: e.reciprocal(rs3[:, 0:bw], rs3[:, 0:bw]), reads=["ang0"], writes=["ang0"])
                for m in range(4):
                    s.op("dve", lambda e: e.scalar_tensor_tensor(mergedT[:, m, b0:b0 + bw], sgg[m][:, 0:bw], vec4[:, V_GOS, m:m + 1], rs3[:, 0:bw], ALU.mult, ALU.mult),
                         reads=[SGN[m], "vec4", "ang0"], writes=["mergedT"])
            if dbg:
                d_m = dbg_out("mergedT", [128, 8, NTOK])
                dtmp2 = sbt(es, "dtmp2", [128, 8, NTOK])
                s.op("dve", lambda e: e.tensor_copy(dtmp2[:], mergedT[:]), reads=["mergedT"], writes=["dtmp2"])
                s.dma("sp", d_m, dtmp2[:], "dbg1", reads=["dtmp2"], writes=["d_m"])

        late = ExitStack()
        glob.enter_context(late)
        s.op("pool", lambda e: e.memset(logit[:], 0.0), writes=["logit"])
        with phase() as es:
            md = load_mod(es, [2, 3, 4], ["gt1", "sh2", "sc2"])
            gffn = sbt(es, "gffn", [128, D])
            s.dma("sp", gffn[:], I["g_ffn"].to_broadcast([128, D]), "gf", writes=["gffn"])
            for (tl, nm, R) in ((md["sc2"][0], "modP_sc2", 128), (md["sc2"][1], "modS_sc2", 64)):
                s.op("dve", lambda e: e.scalar_tensor_tensor(tl[0:R, :], tl[0:R, :], 1.0, gffn[0:R, :], ALU.add, ALU.mult),
                     reads=[nm, "gffn"], writes=[nm])
            w_out_b = sbt(es, "w_out_b", [128, 8, D], BF16)
            s.dma("pool", w_out_b[:], I["w_out"].rearrange("(k p) n -> p k n", p=128), "wout", writes=["w_out_b"])
            wrt = sbt(es, "wrt", [128, 8, 36])
            s.dma("sp", wrt[:], I["w_rt"].rearrange("(k p) n -> p k n", p=128), "wrt", writes=["wrt"])
            brt = sbt(es, "brt", [128, 36])
            s.dma("sp", brt[:], I["b_rt"].to_broadcast([128, 36]), "brt", writes=["brt"])
            xt = [sbt(es, "xtD%d" % i, [128, D]) for i in range(3)]
            x1 = [sbt(es, "x1D%d" % i, [128, D]) for i in range(3)]
            n2 = [sbt(es, "n2D%d" % i, [128, D]) for i in range(3)]
            n2Tf = [sbt(es, "n2Tf%d" % i, [128, 8, 128]) for i in range(3)]
            n2Tb = [sbt(es, "n2Tb%d" % i, [128, D], BF16) for i in range(3)]
            junk = sbt(es, "junkD", [128, D])
            ssq = sbt(es, "ssqD", [128, NT]); rstd = sbt(es, "rstdD", [128, NT])
            mo = [pst(es, "moD%d" % i, [128, 512]) for i in range(4)]
            tpp = [pst(es, "tpD%d" % i, [128, 8, 128]) for i in range(1)]
            lp = pst(es, "lpD", [128, 36])
            def stage_a(i):
                R = tile_rows(i)
                c0 = i * 128
                w = i % 3
                pi_ = 0 if i < 16 else 1
                xb, xn = xt[w], "xtD%d" % w
                s.dma("sp", xb[0:R, :], I["x_all"][c0:c0 + R, :], xn, writes=[xn])
                for h in range(2):
                    mb, mn = mo[(i % 2) * 2 + h], "moD%d" % ((i % 2) * 2 + h)
                    for k in range(8):
                        s.op("pe", lambda e: e.matmul(mb[0:R, :], mergedT[:, k, c0:c0 + R], w_out_b[:, k, h * 512:(h + 1) * 512], start=(k == 0), stop=(k == 7)),
                             reads=["mergedT", "w_out_b"], writes=[mn], signal=(k == 7))
                    pfx = "modP_" if i < 16 else "modS_"
                    s.op("dve", lambda e: e.tensor_tensor(x1[w][0:R, h * 512:(h + 1) * 512], mb[0:R, :], md["gt1"][pi_][0:R, h * 512:(h + 1) * 512], ALU.mult),
                         reads=[mn, pfx + "gt1"], writes=["x1D%d" % w])
                s.op("dve", lambda e: e.tensor_tensor(x1[w][0:R, :], x1[w][0:R, :], xb[0:R, :], ALU.add), reads=["x1D%d" % w, xn], writes=["x1D%d" % w])
                s.dma("sp", x1_scr[c0:c0 + R, :], x1[w][0:R, :], "x1st%d" % w, reads=["x1D%d" % w], writes=["x1_scr%d" % i])
                s.op("act", lambda e: e.activation(junk[0:R, :], x1[w][0:R, :], AF.Square, accum_out=ssq[0:R, i:i + 1]),
                     reads=["x1D%d" % w], writes=["junkD", "ssqD"])
                s.op("act", lambda e: e.activation(rstd[0:R, i:i + 1], ssq[0:R, i:i + 1], AF.Sqrt, scale=1.0 / D, bias=epsc[0:R, :]),
                     reads=["ssqD", "epsc"], writes=["rstdD"])
                s.op("dve", lambda e: e.reciprocal(rstd[0:R, i:i + 1], rstd[0:R, i:i + 1]), reads=["rstdD"], writes=["rstdD"])
                s.op("dve", lambda e: e.scalar_tensor_tensor(n2[w][0:R, :], x1[w][0:R, :], rstd[0:R, i:i + 1], md["sc2"][pi_][0:R, :], ALU.mult, ALU.mult),
                     reads=["x1D%d" % w, "rstdD", pfx + "sc2"], writes=["n2D%d" % w])
                s.op("dve", lambda e: e.tensor_tensor(n2[w][0:R, :], n2[w][0:R, :], md["sh2"][pi_][0:R, :], ALU.add),
                     reads=["n2D%d" % w, pfx + "sh2"], writes=["n2D%d" % w])

            def stage_b(i):
                R = tile_rows(i)
                c0 = i * 128
                w = i % 3
                tpb = tpp[0]
                for k in range(8):
                    s.op("pe", lambda e: e.transpose(tpb[:, k, 0:R], n2[w][0:R, k * 128:(k + 1) * 128], ident[0:R, 0:R]),
                         reads=["n2D%d" % w, "ident"], writes=["tpD0"], signal=(k == 7))
                s.op("act", lambda e: e.copy(n2Tb[w][0:R, :], n2[w][0:R, :]), reads=["n2D%d" % w], writes=["n2Tb%d" % w])
                s.dma("sp", n2_scr[c0:c0 + R, :], n2Tb[w][0:R, :], "n2st%d" % w, reads=["n2Tb%d" % w], writes=["n2_scr%d" % i])
                s.op("dve", lambda e: e.tensor_copy(n2Tf[w][:, :, 0:R], tpb[:, :, 0:R]), reads=["tpD0"], writes=["n2Tf%d" % w])

            def stage_c(i):
                R = tile_rows(i)
                w = i % 3
                for k in range(8):
                    s.op("pe", lambda e: e.matmul(lp[0:R, :], n2Tf[w][:, k, 0:R], wrt[:, k, :], start=(k == 0), stop=(k == 7)),
                         reads=["n2Tf%d" % w, "wrt"], writes=["lpD"], signal=(k == 7))
                s.op("dve", lambda e: e.tensor_tensor(logit[0:R, i, :], lp[0:R, :], brt[0:R, :], ALU.add), reads=["lpD", "brt"], writes=["logit"])


            for i in range(NT + 2):
                if i < NT:
                    stage_a(i)
                if 1 <= i <= NT:
                    stage_b(i - 1)
                if i >= 2:
                    stage_c(i - 2)
        s.barrier()
        mix_es.close()
        slot_i = sbt(late, "slot_i", [128, 2, NT], I32)
        wk = sbt(late, "wk", [128, 2, NT])
        NW = 3
        w1b = [sbt(late, "w1b%d" % i, [128, 8, DE], BF16) for i in range(NW)]
        w3b = [sbt(late, "w3b%d" % i, [128, 8, DE], BF16) for i in range(NW)]
        w2b = [sbt(late, "w2b%d" % i, [128, 4, D], BF16) for i in range(NW)]

        def load_w(e_):
            w = e_ % NW
            s.dma("pool", w1b[w][:], I["w1"][e_].rearrange("(k p) n -> p k n", p=128), "w1b%d" % w, writes=["w1b%d" % w])
            s.dma("pool", w3b[w][:], I["w3"][e_].rearrange("(k p) n -> p k n", p=128), "w3b%d" % w, writes=["w3b%d" % w])
            s.dma("pool", w2b[w][:], I["w2"][e_].rearrange("(k p) n -> p k n", p=128), "w2b%d" % w, writes=["w2b%d" % w])

        for e_ in range(min(NW - 1, n_exp)):
            load_w(e_)
        with phase() as es:
            RT = "route"
            A3 = [128, NT]
            mx = sbt(es, "mx", A3); sm = sbt(es, "sm", A3); ptop = sbt(es, "ptop", A3)
            eg = sbt(es, "eg", [128, NT, 4]); ohg = sbt(es, "ohg", [128, NT, 4])
            sel = sbt(es, "sel", [128, NT, 8]); tmp8 = sbt(es, "tmp8", [128, NT, 8])
            m1 = sbt(es, "m1", A3); m2 = sbt(es, "m2", A3)
            k1 = sbt(es, "k1", [128, NT, 8]); k2 = sbt(es, "k2", [128, NT, 8])
            oh = [sbt(es, "oh%d" % i, [128, NT, 32]) for i in range(2)]
            mk = sbt(es, "mk", [128, NT, 32]); mkb = sbt(es, "mkb", [128, NT * 32], BF16)
            pos = sbt(es, "pos", [128, NT, 32]); offs = sbt(es, "offs", [128, NT, 32])
            t32 = sbt(es, "t32", [128, NT, 32])
            sl = sbt(es, "sl", [128, 2, NT]); pk = sbt(es, "pk", [128, 2, NT])
            ltri = sbt(es, "ltri", [128, 128]); ltrib = sbt(es, "ltrib", [128, 128], BF16)
            onesb = sbt(es, "onesb", [128, 128], BF16)
            eoff = sbt(es, "eoff", [128, 32])
            cup = pst(es, "cup", [128, 2, 512])
            top = pst(es, "top", [128, 2, 512])
            s.dma("sp", ltri[:], I["c_ltri"], "e0", writes=["ltri"])
            s.dma("sp", eoff[:], I["c_eoff"], "e1", writes=["eoff"])
            s.op("dve", lambda e: e.tensor_copy(ltrib[:], ltri[:]), reads=["ltri"], writes=["ltrib"])
            s.op("dve", lambda e: e.tensor_copy(onesb[:], ones32[:]), reads=["ones32"], writes=["onesb"])

            def rv(fn, rd=(), wr=()):
                s.op("dve", fn, reads=[RT] + list(rd), writes=[RT] + list(wr))

            def ra(fn):
                s.op("act", fn, reads=[RT], writes=[RT])

            def bc(ap2, n):
                return ap2.unsqueeze(2).to_broadcast([128, NT, n])

            lg = logit[:, :, 0:4]
            rv(lambda e: e.tensor_reduce(mx[:], lg, AX.X, ALU.max), rd=["logit"])
            rv(lambda e: e.tensor_tensor(eg[:], lg, bc(mx[:], 4), ALU.subtract), rd=["logit"])
            rv(lambda e: e.tensor_single_scalar(ohg[:], eg[:], 0.0, ALU.is_ge))
            ra(lambda e: e.activation(eg[:], eg[:], AF.Exp))
            rv(lambda e: e.tensor_reduce(sm[:], eg[:], AX.X, ALU.add))
            rv(lambda e: e.reciprocal(ptop[:], sm[:]))
            for g_ in range(4):
                le = logit[:, :, 4 + 8 * g_:12 + 8 * g_]
                if g_ == 0:
                    rv(lambda e: e.tensor_tensor(sel[:], le, bc(ohg[:, :, 0], 8), ALU.mult), rd=["logit"])
                else:
                    rv(lambda e: e.tensor_tensor(tmp8[:], le, bc(ohg[:, :, g_], 8), ALU.mult), rd=["logit"])
                    rv(lambda e: e.tensor_tensor(sel[:], sel[:], tmp8[:], ALU.add))
            rv(lambda e: e.tensor_reduce(m1[:], sel[:], AX.X, ALU.max))
            rv(lambda e: e.tensor_tensor(k1[:], sel[:], bc(m1[:], 8), ALU.is_ge))
            rv(lambda e: e.scalar_tensor_tensor(tmp8[:], k1[:], -1e30, sel[:], ALU.mult, ALU.add))
            rv(lambda e: e.tensor_reduce(m2[:], tmp8[:], AX.X, ALU.max))
            rv(lambda e: e.tensor_tensor(k2[:], tmp8[:], bc(m2[:], 8), ALU.is_ge))
            rv(lambda e: e.tensor_tensor(wk[:, 0, :], m2[:], m1[:], ALU.subtract), wr=["wk"])
            s.op("act", lambda e: e.activation(wk[:, 0, :], wk[:, 0, :], AF.Exp), reads=[RT, "wk"], writes=[RT, "wk"])
            rv(lambda e: e.tensor_scalar(wk[:, 0, :], wk[:, 0, :], 1.0, None, ALU.add), wr=["wk"])
            rv(lambda e: e.reciprocal(wk[:, 0, :], wk[:, 0, :]), wr=["wk"])
            rv(lambda e: e.tensor_tensor(wk[:, 0, :], wk[:, 0, :], ptop[:], ALU.mult), wr=["wk"])
            rv(lambda e: e.tensor_tensor(wk[:, 1, :], ptop[:], wk[:, 0, :], ALU.subtract), wr=["wk"])
            for g_ in range(4):
                rv(lambda e: e.tensor_tensor(oh[0][:, :, 8 * g_:8 * g_ + 8], k1[:], bc(ohg[:, :, g_], 8), ALU.mult))
                rv(lambda e: e.tensor_tensor(oh[1][:, :, 8 * g_:8 * g_ + 8], k2[:], bc(ohg[:, :, g_], 8), ALU.mult))
            rv(lambda e: e.tensor_tensor(mk[:], oh[0][:], oh[1][:], ALU.add))
            rv(lambda e: e.memset(mk[64:128, NT - 1, :], 0.0))
            rv(lambda e: e.tensor_copy(mkb[:], mk[:].rearrange("p i e -> p (i e)")), wr=["mkb"])
            NH = NT * 32 // 2
            for h_ in range(2):
                s.op("pe", lambda e: e.matmul(cup[:, h_, 0:NH], ltrib[:], mkb[:, h_ * NH:(h_ + 1) * NH], start=True, stop=True),
                     reads=["ltrib", "mkb"], writes=["cup"])
                s.op("pe", lambda e: e.matmul(top[:, h_, 0:NH], onesb[:], mkb[:, h_ * NH:(h_ + 1) * NH], start=True, stop=True),
                     reads=["onesb", "mkb"], writes=["top"])
            posf = pos[:].rearrange("p i e -> p (i e)")
            t32f = t32[:].rearrange("p i e -> p (i e)")
            for h_ in range(2):
                rv(lambda e: e.tensor_copy(posf[:, h_ * NH:(h_ + 1) * NH], cup[:, h_, 0:NH]), rd=["cup"])
                rv(lambda e: e.tensor_copy(t32f[:, h_ * NH:(h_ + 1) * NH], top[:, h_, 0:NH]), rd=["top"])
            rv(lambda e: e.memset(offs[:, 0, :], 0.0))
            for i in range(1, NT):
                rv(lambda e: e.tensor_tensor(offs[:, i, :], offs[:, i - 1, :], t32[:, i - 1, :], ALU.add))
            rv(lambda e: e.tensor_tensor(pos[:], pos[:], offs[:], ALU.add))
            for k_ in range(2):
                rv(lambda e: e.tensor_tensor(t32[:], oh[k_][:], pos[:], ALU.mult))
                rv(lambda e: e.tensor_reduce(pk[:, k_, :], t32[:], AX.X, ALU.add))
                rv(lambda e: e.tensor_tensor(t32[:], oh[k_][:], eoff[:].unsqueeze(1).to_broadcast([128, NT, 32]), ALU.mult), rd=["eoff"])
                rv(lambda e: e.tensor_reduce(sl[:, k_, :], t32[:], AX.X, ALU.add))
            rv(lambda e: e.tensor_scalar(pk[:], pk[:], float(CAP - 1), None, ALU.min))
            rv(lambda e: e.tensor_tensor(sl[:], sl[:], pk[:], ALU.add))
            rv(lambda e: e.tensor_copy(slot_i[:], sl[:]), wr=["slot_i"])
            if dbg:
                d_c = dbg_out("slots", [128, 2, NT])
                s.dma("sp", d_c, sl[:], "dbg2", reads=[RT], writes=["d_c"])
            nb2 = [sbt(es, "nb2_%d" % i, [128, D], BF16) for i in range(4)]
            for i in range(NT):
                R = tile_rows(i)
                w = i % 4
                s.dma("sp", nb2[w][0:R, :], n2_scr[i * 128:i * 128 + R, :], "nb2ld%d" % w, reads=["n2_scr%d" % i], writes=["nb2_%d" % w])
                for k_ in range(2):
                    s.dma_custom("pool", lambda e: e.indirect_dma_start(
                        out=xs_scr[:, :], out_offset=bass.IndirectOffsetOnAxis(ap=slot_i[0:R, k_, i:i + 1], axis=0),
                        in_=nb2[w][0:R, :], in_offset=None),
                        "sc%d" % w, reads=["nb2_%d" % w, "slot_i", "xs_zf"], writes=["xs_scr_%d" % w])

        with phase() as es:
            xe = [sbt(es, "xe%d" % i, [128, NJ, D], BF16) for i in range(2)]
            xT = [sbt(es, "xT%d" % i, [128, 8, CAP], BF16) for i in range(2)]
            hid = [sbt(es, "hid%d" % i, [128, 4, CAP], BF16) for i in range(2)]
            sgs = [sbt(es, "sgs%d" % i, [128, CAP]) for i in range(2)]
            yo = [sbt(es, "yo%d" % i, [128, D]) for i in range(2)]
            gup = [pst(es, "gup%d" % i, [128, 512]) for i in range(4)]
            dnp = [pst(es, "dnp%d" % i, [128, 512]) for i in range(2)]
            tpx = [pst(es, "tpx%d" % i, [128, 8, 128], BF16) for i in range(2)]
            cnt_g = 0
            cnt_d = 0
            cnt_t = 0
            cnt_y = 0

            def load_x(e_):
                v = e_ % 2
                s.dma("sp", xe[v][:], xs_scr[e_ * CAP:(e_ + 1) * CAP, :].rearrange("(j p) d -> p j d", p=128), "xe%d" % v,
                      reads=["xs_scr_0", "xs_scr_1", "xs_zf"], writes=["xe%d" % v])

            def do_T(e_):
                nonlocal cnt_t
                v = e_ % 2
                for j in range(NJ):
                    tb_ = tpx[cnt_t % 2]
                    tn = "tpx%d" % (cnt_t % 2)
                    cnt_t += 1
                    for k in range(8):
                        s.op("pe", lambda e: e.transpose(tb_[:, k, :], xe[v][:, j, k * 128:(k + 1) * 128], identb[:]),
                             reads=["xe%d" % v, "identb"], writes=[tn], signal=(k == 7))
                    if j % 2 == 0:
                        s.op("act", lambda e: e.copy(xT[v][:, :, j * 128:(j + 1) * 128], tb_[:]), reads=[tn], writes=["xT%d" % v])
                    else:
                        s.op("dve", lambda e: e.tensor_copy(xT[v][:, :, j * 128:(j + 1) * 128], tb_[:]), reads=[tn], writes=["xT%d" % v])

            def do_GU(e_):
                nonlocal cnt_g
                w = e_ % NW
                v = e_ % 2
                for m in range(4):
                    gi = cnt_g % 2
                    cnt_g += 1
                    gp, gpn = gup[gi * 2], "gup%d" % (gi * 2)
                    up, upn = gup[gi * 2 + 1], "gup%d" % (gi * 2 + 1)
                    for k in range(8):
                        s.op("pe", lambda e: e.matmul(gp[:, 0:CAP], w1b[w][:, k, m * 128:(m + 1) * 128], xT[v][:, k, :], start=(k == 0), stop=(k == 7)),
                             reads=["w1b%d" % w, "xT%d" % v], writes=[gpn], signal=(k == 7))
                    for k in range(8):
                        s.op("pe", lambda e: e.matmul(up[:, 0:CAP], w3b[w][:, k, m * 128:(m + 1) * 128], xT[v][:, k, :], start=(k == 0), stop=(k == 7)),
                             reads=["w3b%d" % w, "xT%d" % v], writes=[upn], signal=(k == 7))
                    s.op("act", lambda e: e.activation(sgs[gi][:, :], gp[:, 0:CAP], AF.Silu), reads=[gpn], writes=["sgs%d" % gi])
                    s.op("dve", lambda e: e.tensor_tensor(hid[v][:, m, :], up[:, 0:CAP], sgs[gi][:, :], ALU.mult), reads=[upn, "sgs%d" % gi], writes=["hid%d" % v])

            def do_DN(e_):
                nonlocal cnt_d, cnt_y
                w = e_ % NW
                v = e_ % 2
                for j in range(NJ):
                    yv = cnt_y % 2
                    cnt_y += 1
                    for h in range(2):
                        di = cnt_d % 2
                        cnt_d += 1
                        for m in range(4):
                            s.op("pe", lambda e: e.matmul(dnp[di][:, :], hid[v][:, m, j * 128:(j + 1) * 128], w2b[w][:, m, h * 512:(h + 1) * 512], start=(m == 0), stop=(m == 3)),
                                 reads=["hid%d" % v, "w2b%d" % w], writes=["dnp%d" % di], signal=(m == 3))
                        if h == 0:
                            s.op("act", lambda e: e.copy(yo[yv][:, 0:512], dnp[di][:, :]), reads=["dnp%d" % di], writes=["yo%d" % yv])
                        else:
                            s.op("dve", lambda e: e.tensor_copy(yo[yv][:, 512:1024], dnp[di][:, :]), reads=["dnp%d" % di], writes=["yo%d" % yv])
                    r0 = e_ * CAP + j * 128
                    s.dma("sp", ys_scr[r0:r0 + 128, :], yo[yv][:], "ysst%d" % yv, reads=["yo%d" % yv], writes=["ys_scr_%d" % yv])

            load_x(0)
            if n_exp > 1:
                load_x(1)
            do_T(0)
            for e_ in range(n_exp):
                if e_ + NW - 1 < n_exp:
                    load_w(e_ + NW - 1)
                do_GU(e_)
                if e_ + 1 < n_exp:
                    do_T(e_ + 1)
                if e_ + 2 < n_exp:
                    load_x(e_ + 2)
                do_DN(e_)

        with phase() as es:
            md = load_mod(es, [5], ["gt2"])
            gfin = sbt(es, "gfin", [128, D])
            s.dma("sp", gfin[:], I["g_fin"].to_broadcast([128, D]), "gfi", writes=["gfin"])
            x1 = [sbt(es, "x1G%d" % i, [128, D]) for i in range(6)]
            g0 = [sbt(es, "g0G%d" % i, [128, D]) for i in range(6)]
            g1 = [sbt(es, "g1G%d" % i, [128, D]) for i in range(6)]
            yo = [sbt(es, "yoG%d" % i, [128, D]) for i in range(6)]
            junk = sbt(es, "junkG", [128, D])
            ssq = sbt(es, "ssqG", [128, NT]); rstd = sbt(es, "rstdG", [128, NT])
            for i in range(NT):
                R = tile_rows(i)
                c0 = i * 128
                w = i % 6
                pi_ = 0 if i < 16 else 1
                pfx = "modP_" if i < 16 else "modS_"
                s.dma("sp", x1[w][0:R, :], x1_scr[c0:c0 + R, :], "x1ld%d" % w, reads=["x1_scr%d" % i], writes=["x1G%d" % w])
                for (gt, gn, k_) in ((g0[w], "g0G%d" % w, 0), (g1[w], "g1G%d" % w, 1)):
                    s.dma_custom("pool", lambda e: e.indirect_dma_start(
                        out=gt[0:R, :], out_offset=None, in_=ys_scr[:, :],
                        in_offset=bass.IndirectOffsetOnAxis(ap=slot_i[0:R, k_, i:i + 1], axis=0)),
                        "gG%d" % w, reads=["ys_scr_0", "ys_scr_1", "slot_i"], writes=[gn])
                s.op("act", lambda e: e.activation(g0[w][0:R, :], g0[w][0:R, :], AF.Copy, scale=wk[0:R, 0, i:i + 1]),
                     reads=["g0G%d" % w, "g1G%d" % w, "wk"], writes=["g0G%d" % w])
                s.op("dve", lambda e: e.scalar_tensor_tensor(g0[w][0:R, :], g1[w][0:R, :], wk[0:R, 1, i:i + 1], g0[w][0:R, :], ALU.mult, ALU.add),
                     reads=["g1G%d" % w, "g0G%d" % w, "wk"], writes=["g0G%d" % w])
                s.op("dve", lambda e: e.tensor_tensor(yo[w][0:R, :], g0[w][0:R, :], md["gt2"][pi_][0:R, :], ALU.mult),
                     reads=["g0G%d" % w, pfx + "gt2"], writes=["yoG%d" % w])
                s.op("dve", lambda e: e.tensor_tensor(x1[w][0:R, :], x1[w][0:R, :], yo[w][0:R, :], ALU.add),
                     reads=["x1G%d" % w, "yoG%d" % w], writes=["x1G%d" % w])
                s.op("act", lambda e: e.activation(junk[0:R, :], x1[w][0:R, :], AF.Square, accum_out=ssq[0:R, i:i + 1]),
                     reads=["x1G%d" % w], writes=["junkG", "ssqG"])
                s.op("act", lambda e: e.activation(rstd[0:R, i:i + 1], ssq[0:R, i:i + 1], AF.Sqrt, scale=1.0 / D, bias=epsc[0:R, :]),
                     reads=["ssqG", "epsc"], writes=["rstdG"])
                s.op("dve", lambda e: e.reciprocal(rstd[0:R, i:i + 1], rstd[0:R, i:i + 1]), reads=["rstdG"], writes=["rstdG"])
                s.op("dve", lambda e: e.scalar_tensor_tensor(yo[w][0:R, :], x1[w][0:R, :], rstd[0:R, i:i + 1], gfin[0:R, :], ALU.mult, ALU.mult),
                     reads=["x1G%d" % w, "rstdG", "gfin"], writes=["yoG%d" % w])
                s.dma("sp", O["y_all"][c0:c0 + R, :], yo[w][0:R, :], "yst%d" % w, reads=["yoG%d" % w], writes=["o_y%d" % i])
        s.finish("sp")
    return nc, list(DBG.keys())


def _consts():
    ident = np.eye(128, dtype=np.float32)
    swp = np.zeros((128, 128), np.float32)
    for k in range(64):
        swp[k, k + 64] = 1.0
        swp[k + 64, k] = 1.0
    rowmask = np.zeros((128, 8), np.float32)
    for p in range(128):
        rowmask[p, p // 16] = 1.0
    sgn = np.ones((128, 1), np.float32)
    sgn[64:] = -1.0
    j = np.concatenate([np.arange(1, 513), np.tile(np.arange(1, TS + 1), NS)]).astype(np.float32)
    jidx = np.broadcast_to(j[None, :], (128, 576)).copy()
    m01 = np.ones((128, NS, TS), np.float32)
    m01[:, :, 0] = 0.0
    ltri = np.triu(np.ones((128, 128), np.float32), 1)
    eoff = np.broadcast_to((np.arange(32, dtype=np.float32) * CAP)[None, :], (128, 32)).copy()
    return dict(c_ident=ident, c_swp=swp, c_rowmask=rowmask, c_sgn=sgn, c_jidx=jidx, c_m01=m01.reshape(128, 64),
                c_ltri=ltri, c_eoff=eoff)


def make_in_maps(inp):
    f = lambda a: np.ascontiguousarray(np.asarray(a, dtype=np.float32))
    sh = {}
    sh["w_ada"] = f(inp["w_ada"][0]); sh["b_ada"] = f(inp["b_ada"][0][None, :])
    sh["g_mix"] = f(inp["g_norm_mix"][0][None, :]); sh["g_ffn"] = f(inp["g_norm_ffn"][0][None, :])
    sh["g_fin"] = f(inp["g_final"][None, :])
    sh["w_in"] = f(inp["w_in"][0]); sh["w_out"] = f(inp["w_out"][0]); sh["w_glu"] = f(inp["w_ssm_glu"][0])
    are = np.asarray(inp["ssm_a_re"][0]).T; aim = np.asarray(inp["ssm_a_im"][0]).T
    sh["are2"] = f(np.concatenate([are, are], 0)); sh["aim2"] = f(np.concatenate([aim, aim], 0))
    sh["ldt2"] = f(np.broadcast_to(np.asarray(inp["ssm_log_dt"][0])[None, :], (128, G)))
    br = np.asarray(inp["ssm_b_re"][0]).transpose(1, 0, 2); bi = np.asarray(inp["ssm_b_im"][0]).transpose(1, 0, 2)
    sh["bx1"] = f(np.concatenate([br, bi], 0)); sh["bx2"] = f(np.concatenate([bi, br], 0))
    cr = np.asarray(inp["ssm_c_re"][0]).transpose(2, 0, 1); ci = np.asarray(inp["ssm_c_im"][0]).transpose(2, 0, 1)
    sh["ct1"] = f(np.concatenate([cr, ci], 0)); sh["ct2"] = f(np.concatenate([ci, cr], 0))
    vecs = [np.asarray(inp["ssm_d"][0]).reshape(512), inp["b_ssm_glu"][0], inp["b_dw"][0], inp["ln_conv_g"][0],
            inp["ln_conv_b"][0], inp["g_out_ssm"][0], inp["g_out_conv"][0]]
    sh["vec4"] = f(np.stack([np.asarray(v).reshape(4, 128).T for v in vecs], 1))
    sh["wdwT"] = f(np.asarray(inp["w_dw"][0]).reshape(CW, 4, 128).transpose(2, 1, 0))
    sh["w_rt"] = f(np.concatenate([np.asarray(inp["w_router_grp"][0]), np.asarray(inp["w_router_exp"][0]).reshape(D, 32)], 1))
    sh["b_rt"] = f(np.concatenate([np.asarray(inp["b_router_grp"][0]), np.asarray(inp["b_router_exp"][0]).reshape(32)])[None, :])
    sh["w1"] = f(inp["w_exp_gate"][0]); sh["w3"] = f(inp["w_exp_up"][0]); sh["w2"] = f(inp["w_exp_down"][0])
    sh.update(_consts())
    xp = np.asarray(inp["x_prompt"]); xs = np.asarray(inp["x_sample"])
    cp = np.asarray(inp["c_prompt"]); cs = np.asarray(inp["c_sample"])
    sre = np.asarray(inp["state_ssm_re"][0]); sim = np.asarray(inp["state_ssm_im"][0])
    cc = np.asarray(inp["cache_conv"][0])
    maps = []
    for c in range(NCORES):
        m = dict(sh)
        bs = slice(c * NS, (c + 1) * NS)
        m["x_all"] = f(np.concatenate([xp[c], xs[bs].reshape(NS * TS, D)], 0))
        m["c_all"] = f(np.concatenate([cp[c:c + 1], cs[bs]], 0))
        m["h0s"] = f(np.concatenate([sre[bs].transpose(2, 1, 0), sim[bs].transpose(2, 1, 0)], 0))
        m["cachef"] = f(cc[bs].reshape(NS, CB, 4, 128).transpose(3, 2, 0, 1))
        maps.append(m)
    return maps


def assemble(results, inp):
    y_p = np.zeros((8, SEQ, D), np.float32); y_s = np.zeros((128, TS, D), np.float32)
    pre = np.zeros((1, 8, G, P), np.float32); pim = np.zeros((1, 8, G, P), np.float32)
    pbuf = np.zeros((1, 8, CB, 512), np.float32)
    sre = np.zeros((1, 128, G, P), np.float32); sim = np.zeros((1, 128, G, P), np.float32)
    sbuf = np.zeros((1, 128, CB, 512), np.float32)
    for c in range(NCORES):
        r = results[c]
        bs = slice(c * NS, (c + 1) * NS)
        y_p[c] = r["y_all"][:SEQ]
        y_s[bs] = r["y_all"][SEQ:].reshape(NS, TS, D)
        stp = r["st_p"]
        pre[0, c] = stp[:64].T; pim[0, c] = stp[64:].T
        sts = r["st_s"]
        sre[0, bs] = sts[:64].transpose(2, 1, 0); sim[0, bs] = sts[64:].transpose(2, 1, 0)
        pbuf[0, c] = r["cc_p"].transpose(2, 1, 0).reshape(CB, 512)
        sbuf[0, bs] = r["cc_s"].transpose(2, 3, 1, 0).reshape(NS, CB, 512)
    return y_p, y_s, pre, pim, pbuf, sre, sim, sbuf


_NC_CACHE = {}


def kernel(**inputs):
    if "nc" not in _NC_CACHE:
        _NC_CACHE["nc"] = build()[0]
    nc = _NC_CACHE["nc"]
    maps = make_in_maps(inputs)
    res = run_bass_kernel_spmd(nc, maps, core_ids=list(range(NCORES)))
    return assemble(res.results, inputs)
```
